# Optimizing a Trainium2 kernel written in Bass

```python
import jax
import jax.numpy as jnp
from jax import lax
import numpy as np

D_MODEL = 1024
BATCH = 32
SEQ = 2048
DEPTH = 4

CHUNK = 64
Q_BLOCK = 128
ROPE_THETA = 10000.0
NEG_INF = -1e30
LN_EPS = 1e-5
RMS_EPS = 1e-6

H_MLA = 8
D_NOPE = 64
D_ROPE = 32
D_V_MLA = 64
Q_LORA = 384
KV_LORA = 256

H_DSA = 8
D_DSA = 64
H_IDX = 4
D_IDX = 64
TOPK_MAX = 256

H_FOX = 16
D_FOX = 64
FORGET_BIAS = 2.0

N_GROUPS = 4
EXPERTS_PER_GROUP = 4
N_EXPERTS = N_GROUPS * EXPERTS_PER_GROUP
D_EXPERT = 512
TOP_K_INNER = 2

DN_ALPHA = (2 * DEPTH) ** 0.25
DN_BETA = (8 * DEPTH) ** -0.25

N_EVEN = (DEPTH + 1) // 2
N_ODD = DEPTH // 2

EV_SPLITS = (Q_LORA, KV_LORA, D_ROPE, H_DSA * D_DSA, D_DSA, D_DSA, H_IDX * D_IDX, D_IDX, H_IDX)
EV_IN = sum(EV_SPLITS)
EV_OUT = H_MLA * D_V_MLA + H_DSA * D_DSA
OD_SPLITS = (H_FOX * D_FOX, H_FOX * D_FOX, H_FOX * D_FOX, H_FOX)
OD_IN = sum(OD_SPLITS)
OD_OUT = H_FOX * D_FOX

kernel_name = 'hybrid_mla_dsa_fox_hmoe_deepnorm'


def _split(h, sizes):
    offs = np.cumsum(sizes)[:-1].tolist()
    return jnp.split(h, offs, axis=-1)


def _layer_norm(x, g, b):
    xf = x.astype(jnp.float32)
    mu = jnp.mean(xf, axis=-1, keepdims=True)
    var = jnp.mean(jnp.square(xf - mu), axis=-1, keepdims=True)
    y = (xf - mu) * lax.rsqrt(var + LN_EPS)
    return (y * g.astype(jnp.float32) + b.astype(jnp.float32)).astype(x.dtype)


def _rms_norm(x, g):
    xf = x.astype(jnp.float32)
    y = xf * lax.rsqrt(jnp.mean(jnp.square(xf), axis=-1, keepdims=True) + RMS_EPS)
    return (y * g.astype(jnp.float32)).astype(x.dtype)


def _rope(x, pos):
    d = x.shape[-1]
    inv = jnp.power(ROPE_THETA, -jnp.arange(0, d, 2, dtype=jnp.float32) / d)
    ang = pos.astype(jnp.float32)[..., None] * inv
    cos = jnp.cos(ang)[:, :, None, :]
    sin = jnp.sin(ang)[:, :, None, :]
    xf = x.astype(jnp.float32)
    x1, x2 = xf[..., : d // 2], xf[..., d // 2:]
    return jnp.concatenate([x1 * cos - x2 * sin, x2 * cos + x1 * sin], axis=-1).astype(x.dtype)


def _mla_attention(q_nope, q_rope, k_nope, k_rope, v):
    S = q_nope.shape[1]
    scale = (D_NOPE + D_ROPE) ** -0.5
    outs = []
    for i in range(S // Q_BLOCK):
        q0, q1 = i * Q_BLOCK, (i + 1) * Q_BLOCK
        s = (jnp.einsum('bqhd,bkhd->bhqk', q_nope[:, q0:q1], k_nope[:, :q1])
             + jnp.einsum('bqhd,bkd->bhqk', q_rope[:, q0:q1], k_rope[:, :q1]))
        s = s.astype(jnp.float32) * scale
        mask = (jnp.arange(q0, q1) // CHUNK)[:, None] >= (jnp.arange(q1) // CHUNK)[None, :]
        p = jax.nn.softmax(jnp.where(mask, s, NEG_INF), axis=-1).astype(v.dtype)
        outs.append(jnp.einsum('bhqk,bkhd->bqhd', p, v[:, :q1]))
    return jnp.concatenate(outs, axis=1)


def _dsa_attention(q, k, v, q_idx, k_idx, w_idx, k_sel):
    B, S, H, D = q.shape
    nb = S // Q_BLOCK
    scale = D ** -0.5
    key_chunk = jnp.arange(S) // CHUNK
    gather = jax.vmap(lambda a, i: a[i])

    def to_blocks(a):
        return a.reshape((B, nb, Q_BLOCK) + a.shape[2:]).swapaxes(0, 1)

    def block(args):
        qb, qib, wib, q0 = args
        q_chunk = (q0 + jnp.arange(Q_BLOCK)) // CHUNK
        adm = q_chunk[:, None] >= key_chunk[None, :]
        rel = jax.nn.relu(jnp.einsum('bqhd,bkd->bqhk', qib, k_idx).astype(jnp.float32))
        score = jnp.einsum('bqhk,bqh->bqk', rel, wib.astype(jnp.float32))
        score = jnp.where(adm[None], score, NEG_INF)
        _, sel = lax.top_k(score, k_sel)
        valid = (sel // CHUNK) <= q_chunk[None, :, None]
        k_g = gather(k, sel)
        v_g = gather(v, sel)
        s = jnp.einsum('bqhd,bqkd->bqhk', qb, k_g).astype(jnp.float32) * scale
        s = jnp.where(valid[:, :, None, :], s, NEG_INF)
        p = jax.nn.softmax(s, axis=-1).astype(v.dtype)
        return jnp.einsum('bqhk,bqkd->bqhd', p, v_g)

    out = lax.map(block, (to_blocks(q), to_blocks(q_idx), to_blocks(w_idx),
                          jnp.arange(nb, dtype=jnp.int32) * Q_BLOCK))
    return out.swapaxes(0, 1).reshape(B, S, H, D)


def _fox_attention(q, k, v, log_f):
    S = q.shape[1]
    scale = q.shape[-1] ** -0.5
    c = jnp.swapaxes(jnp.cumsum(log_f, axis=1), 1, 2)
    outs = []
    for i in range(S // Q_BLOCK):
        q0, q1 = i * Q_BLOCK, (i + 1) * Q_BLOCK
        s = jnp.einsum('bqhd,bkhd->bhqk', q[:, q0:q1], k[:, :q1]).astype(jnp.float32) * scale
        s = s + c[:, :, q0:q1, None] - c[:, :, None, :q1]
        mask = jnp.arange(q0, q1)[:, None] >= jnp.arange(q1)[None, :]
        p = jax.nn.softmax(jnp.where(mask, s, NEG_INF), axis=-1).astype(v.dtype)
        outs.append(jnp.einsum('bhqk,bkhd->bqhd', p, v[:, :q1]))
    return jnp.concatenate(outs, axis=1)


def _even_mixer(x, pos, w_in, g_q, g_kv, w_uq, w_ukv, w_o, k_sel):
    B, S, _ = x.shape
    h = x @ w_in
    c_q, c_kv, k_r, q_b, k_b, v_b, q_i, k_i, w_i = _split(h, EV_SPLITS)
    q = (_rms_norm(c_q, g_q) @ w_uq).reshape(B, S, H_MLA, D_NOPE + D_ROPE)
    q_nope, q_rope = q[..., :D_NOPE], _rope(q[..., D_NOPE:], pos)
    kv = (_rms_norm(c_kv, g_kv) @ w_ukv).reshape(B, S, H_MLA, D_NOPE + D_V_MLA)
    k_nope, v_a = kv[..., :D_NOPE], kv[..., D_NOPE:]
    k_rope = _rope(k_r[:, :, None, :], pos)[:, :, 0]
    o_a = _mla_attention(q_nope, q_rope, k_nope, k_rope, v_a)
    q_b = _rope(q_b.reshape(B, S, H_DSA, D_DSA), pos)
    k_b = _rope(k_b[:, :, None, :], pos)[:, :, 0]
    q_i = _rope(q_i.reshape(B, S, H_IDX, D_IDX), pos)
    k_i = _rope(k_i[:, :, None, :], pos)[:, :, 0]
    o_b = _dsa_attention(q_b, k_b, v_b, q_i, k_i, w_i, k_sel)
    o = jnp.concatenate([o_a, o_b], axis=2).reshape(B, S, EV_OUT)
    return o @ w_o


def _odd_mixer(x, w_in, b_f, w_o):
    B, S, _ = x.shape
    q, k, v, f = _split(x @ w_in, OD_SPLITS)
    q = q.reshape(B, S, H_FOX, D_FOX)
    k = k.reshape(B, S, H_FOX, D_FOX)
    v = v.reshape(B, S, H_FOX, D_FOX)
    log_f = jax.nn.log_sigmoid((f + b_f).astype(jnp.float32))
    o = _fox_attention(q, k, v, log_f).reshape(B, S, OD_OUT)
    return o @ w_o


def _hier_moe(x, w_grp, b_grp, w_sub, b_sub, w_gate, w_up, w_down):
    B, S, D = x.shape
    t = x.reshape(-1, D)
    lg = (t @ w_grp + b_grp).astype(jnp.float32)
    p_grp = jax.nn.softmax(lg, axis=-1)
    g_star = jnp.argmax(lg, axis=-1)
    p_top = jnp.take_along_axis(p_grp, g_star[:, None], axis=1)
    ls = (t @ w_sub + b_sub).astype(jnp.float32).reshape(-1, N_GROUPS, EXPERTS_PER_GROUP)
    ls = jnp.take_along_axis(ls, g_star[:, None, None], axis=1)[:, 0]
    v2, j2 = lax.top_k(ls, TOP_K_INNER)
    w2 = jax.nn.softmax(v2, axis=-1) * p_top
    eid = g_star[:, None] * EXPERTS_PER_GROUP + j2
    gates = jnp.sum(jax.nn.one_hot(eid, N_EXPERTS, dtype=jnp.float32) * w2[..., None],
                    axis=1).astype(x.dtype)
    y = jnp.zeros_like(t)
    for e in range(N_EXPERTS):
        h = jax.nn.silu(t @ w_gate[e]) * (t @ w_up[e])
        y = y + gates[:, e:e + 1] * (h @ w_down[e])
    return y.reshape(B, S, D)


def setup_inputs(seed: int = 0) -> dict:
    key = jax.random.key(seed)
    ks = jax.random.split(key, 24)
    f32 = jnp.float32

    def nrm(k, shape, scale):
        return jax.random.normal(k, shape, f32) * scale

    x = jax.random.normal(ks[0], (BATCH, SEQ, D_MODEL), f32)
    offsets = jax.random.randint(ks[1], (BATCH, 1), 0, 4096, dtype=jnp.int32)
    positions = offsets + jnp.arange(SEQ, dtype=jnp.int32)[None, :]
    ev_w_in = nrm(ks[2], (N_EVEN, D_MODEL, EV_IN), D_MODEL ** -0.5)
    ev_g_q = 1.0 + nrm(ks[3], (N_EVEN, Q_LORA), 0.02)
    ev_g_kv = 1.0 + nrm(ks[4], (N_EVEN, KV_LORA), 0.02)
    ev_w_uq = nrm(ks[5], (N_EVEN, Q_LORA, H_MLA * (D_NOPE + D_ROPE)), Q_LORA ** -0.5)
    ev_w_ukv = nrm(ks[6], (N_EVEN, KV_LORA, H_MLA * (D_NOPE + D_V_MLA)), KV_LORA ** -0.5)
    ev_w_o = nrm(ks[7], (N_EVEN, EV_OUT, D_MODEL), DN_BETA * EV_OUT ** -0.5)
    od_w_in = nrm(ks[8], (N_ODD, D_MODEL, OD_IN), D_MODEL ** -0.5)
    od_b_f = FORGET_BIAS + nrm(ks[9], (N_ODD, H_FOX), 0.1)
    od_w_o = nrm(ks[10], (N_ODD, OD_OUT, D_MODEL), DN_BETA * OD_OUT ** -0.5)
    moe_w_grp = nrm(ks[11], (DEPTH, D_MODEL, N_GROUPS), D_MODEL ** -0.5)
    moe_b_grp = nrm(ks[12], (DEPTH, N_GROUPS), 0.01)
    moe_w_sub = nrm(ks[13], (DEPTH, D_MODEL, N_EXPERTS), D_MODEL ** -0.5)
    moe_b_sub = nrm(ks[14], (DEPTH, N_EXPERTS), 0.01)
    moe_w_gate = nrm(ks[15], (DEPTH, N_EXPERTS, D_MODEL, D_EXPERT), D_MODEL ** -0.5)
    moe_w_up = nrm(ks[16], (DEPTH, N_EXPERTS, D_MODEL, D_EXPERT), D_MODEL ** -0.5)
    moe_w_down = nrm(ks[17], (DEPTH, N_EXPERTS, D_EXPERT, D_MODEL), DN_BETA * D_EXPERT ** -0.5)
    ln1_g = 1.0 + nrm(ks[18], (DEPTH, D_MODEL), 0.02)
    ln1_b = nrm(ks[19], (DEPTH, D_MODEL), 0.02)
    ln2_g = 1.0 + nrm(ks[20], (DEPTH, D_MODEL), 0.02)
    ln2_b = nrm(ks[21], (DEPTH, D_MODEL), 0.02)
    return {
        'x': x, 'positions': positions,
        'ev_w_in': ev_w_in, 'ev_g_q': ev_g_q, 'ev_g_kv': ev_g_kv,
        'ev_w_uq': ev_w_uq, 'ev_w_ukv': ev_w_ukv, 'ev_w_o': ev_w_o,
        'od_w_in': od_w_in, 'od_b_f': od_b_f, 'od_w_o': od_w_o,
        'moe_w_grp': moe_w_grp, 'moe_b_grp': moe_b_grp,
        'moe_w_sub': moe_w_sub, 'moe_b_sub': moe_b_sub,
        'moe_w_gate': moe_w_gate, 'moe_w_up': moe_w_up, 'moe_w_down': moe_w_down,
        'ln1_g': ln1_g, 'ln1_b': ln1_b, 'ln2_g': ln2_g, 'ln2_b': ln2_b,
    }


def reference(x, positions, ev_w_in, ev_g_q, ev_g_kv, ev_w_uq, ev_w_ukv, ev_w_o,
              od_w_in, od_b_f, od_w_o, moe_w_grp, moe_b_grp, moe_w_sub, moe_b_sub,
              moe_w_gate, moe_w_up, moe_w_down, ln1_g, ln1_b, ln2_g, ln2_b):
    S = x.shape[1]
    k_sel = min(TOPK_MAX, S // 4)
    for layer in range(DEPTH):
        j = layer // 2
        if layer % 2 == 0:
            y = _even_mixer(x, positions, ev_w_in[j], ev_g_q[j], ev_g_kv[j],
                            ev_w_uq[j], ev_w_ukv[j], ev_w_o[j], k_sel)
        else:
            y = _odd_mixer(x, od_w_in[j], od_b_f[j], od_w_o[j])
        x = _layer_norm(DN_ALPHA * x + y, ln1_g[layer], ln1_b[layer])
        y = _hier_moe(x, moe_w_grp[layer], moe_b_grp[layer], moe_w_sub[layer], moe_b_sub[layer],
                      moe_w_gate[layer], moe_w_up[layer], moe_w_down[layer])
        x = _layer_norm(DN_ALPHA * x + y, ln2_g[layer], ln2_b[layer])
    return x
```

```python
import numpy as np
import concourse.bass as bass
import concourse.mybir as mybir
from concourse.bass_utils import run_bass_kernel_spmd

F32 = mybir.dt.float32
BF16 = mybir.dt.bfloat16
I32 = mybir.dt.int32
AF = mybir.ActivationFunctionType
ALU = mybir.AluOpType
AX = mybir.AxisListType
DTSZ = {F32: 4, BF16: 2, I32: 4}


class Op:
    __slots__ = ("eng", "fn", "r", "w", "dma", "dsem", "dval", "deps", "tick", "marked", "id", "ep")


class KB:
    def __init__(self):
        self.nc = bass.Bass("TRN2", target_bir_lowering=False)
        self.ops = []
        self.sb_off = 0
        self.sb_max = 0
        self.dma_sems = {}

    def sb(self, name, shape, dt, off=None):
        if not hasattr(self, "arena"):
            self.ARENA = 206 * 1024
            self.arena = self.nc.alloc_sbuf_tensor("arena", [128, self.ARENA // 4], F32)
        nbytes = int(np.prod(shape[1:])) * DTSZ[dt]
        nbytes = (nbytes + 3) // 4 * 4
        if off is None:
            off = self.sb_off
            self.sb_off = (off + nbytes + 63) // 64 * 64
        self.sb_max = max(self.sb_max, off + nbytes)
        assert off % 4 == 0 and off + nbytes <= self.ARENA, (name, off, nbytes)
        ap = self.arena[:, off // 4:(off + nbytes) // 4]
        if dt != F32:
            ap = ap.bitcast(dt)
        n = int(np.prod(shape[1:]))
        ap = ap[:, 0:n]
        if len(shape) == 3:
            ap = ap.rearrange("p (a b) -> p a b", b=shape[2])
        elif len(shape) == 4:
            ap = ap.rearrange("p (a b c) -> p a b c", b=shape[2], c=shape[3])
        if shape[0] != 128:
            ap = ap[0:shape[0]]
        return ap

    def op(self, eng, fn, r=(), w=(), dsem=None):
        o = Op()
        o.eng = eng
        o.fn = fn
        o.r = tuple(r)
        o.w = tuple(w)
        o.dma = dsem is not None
        o.dsem = dsem
        o.dval = 0
        o.deps = ()
        o.tick = 0
        o.marked = False
        o.id = len(self.ops)
        self.ops.append(o)
        return o

    def barrier(self):
        return self.op("barrier", None)

    def epoch(self):
        self.op("barrier", None)
        return self.op("epoch", None)

    def dma(self, q, out, in_, r=(), w=(), dsem="d0"):
        return self.op(q, lambda e: e.dma_start(out=out, in_=in_), r, w, dsem=dsem)

    def emit(self):
        nc = self.nc
        ops = self.ops
        lastw = {}
        readers = {}
        lastop = {}
        lastdma = {}
        for o in ops:
            if o.eng == "epoch":
                lastop = {}
                continue
            if o.eng == "barrier":
                o.deps = list(lastop.values()) + list(lastdma.values())
                for p in o.deps:
                    p.marked = True
                lastw = {}
                readers = {}
                continue
            if o.dma:
                lastdma[o.dsem] = o
            else:
                lastop[o.eng] = o
            deps = {}
            for k in o.r:
                p = lastw.get(k)
                if p is not None:
                    deps[p.id] = "RAW"
            for k in o.w:
                p = lastw.get(k)
                if p is not None and p.id not in deps:
                    deps[p.id] = "WAW"
                for p in readers.get(k, {}).values():
                    if isinstance(p, list):
                        for pp in p:
                            deps.setdefault(pp.id, "WAR")
                    else:
                        deps.setdefault(p.id, "WAR")
            for k in o.r:
                d = readers.setdefault(k, {})
                if o.dma:
                    d.setdefault("dma", []).append(o)
                else:
                    d[o.eng] = o
            for k in o.w:
                lastw[k] = o
                readers[k] = {}
            fd = []
            for pid, kind in deps.items():
                p = ops[pid]
                if p is o:
                    continue
                if (not p.dma) and (not o.dma) and p.eng == o.eng:
                    if o.eng == "pe" or kind != "RAW":
                        continue
                if (not p.dma) and o.dma and p.eng == o.eng and kind != "RAW":
                    pass
                p.marked = True
                fd.append(p)
            o.deps = fd
        cnt = {}
        dcnt = {}
        ep = 0
        self.maxticks = {}
        for o in ops:
            o.ep = ep
            if o.eng == "epoch":
                ep += 1
                cnt = {}
                continue
            if o.eng == "barrier":
                continue
            if o.dma:
                dcnt[o.dsem] = dcnt.get(o.dsem, 0) + 16
                o.dval = dcnt[o.dsem]
            elif o.marked:
                cnt[o.eng] = cnt.get(o.eng, 0) + 1
                o.tick = cnt[o.eng]
                self.maxticks[o.eng] = max(self.maxticks.get(o.eng, 0), o.tick)
        self.nepochs = ep + 1
        engs = {"pe": nc.tensor, "act": nc.scalar, "dve": nc.vector, "pool": nc.gpsimd, "sp": nc.sync}
        sems = {}
        import contextlib
        self._stack = contextlib.ExitStack()
        for o in ops:
            if o.marked and not o.dma and (o.ep, o.eng) not in sems:
                sems[(o.ep, o.eng)] = self._stack.enter_context(nc.semaphore("s_%s_%d" % (o.eng, o.ep)))
        dsems = {}
        for k in dcnt:
            dsems[k] = self._stack.enter_context(nc.semaphore("d_" + str(k)))
        seen = {e: {} for e in engs}
        nwait = 0
        dissued = {}
        for o in ops:
            if o.eng == "epoch":
                continue
            if o.eng == "barrier":
                for en, E in engs.items():
                    sn = seen[en]
                    for p in o.deps:
                        if p.dma:
                            key = ("d", p.dsem); val = p.dval
                        else:
                            if p.eng == en:
                                continue
                            key = ("c", p.eng, p.ep); val = p.tick
                        if sn.get(key, 0) >= val:
                            continue
                        sem = dsems[key[1]] if key[0] == "d" else sems[(key[2], key[1])]
                        E.wait_ge(sem, val)
                        sn[key] = val
                        nwait += 1
                continue
            E = engs[o.eng]
            sn = seen[o.eng]
            need = {}
            for p in o.deps:
                if p.dma:
                    key = ("d", p.dsem)
                    val = dissued[p.dsem]
                else:
                    key = ("c", p.eng, p.ep)
                    val = p.tick
                if sn.get(key, 0) >= val:
                    continue
                if need.get(key, 0) < val:
                    need[key] = val
            for key, val in need.items():
                sem = dsems[key[1]] if key[0] == "d" else sems[(key[2], key[1])]
                E.wait_ge(sem, val)
                sn[key] = val
                nwait += 1
            ins = o.fn(E)
            if o.dma:
                dissued[o.dsem] = o.dval
                ins.then_inc(dsems[o.dsem], 16)
            elif o.marked:
                ins.then_inc(sems[(o.ep, o.eng)], 1)
        self.nwait = nwait
        return nc


import math

D = 1024
ALPHA = 8 ** 0.25
THETA = 10000.0
NIT = 16
C_ID = 0
C_TRI = 128
C_DM = 256
C_TB = 384
C_P2 = C_TB + 2048
C_SEL = C_P2 + 32
C_INV = C_SEL + 16 * 65
CW = C_INV + 4
TWO_PI = 2.0 * math.pi


def make_consts():
    c = np.zeros((128, CW), np.float32)
    c[:, C_ID:C_ID + 128] = np.eye(128)
    s = np.arange(128)[:, None]
    t = np.arange(128)[None, :]
    c[:, C_TRI:C_TRI + 128] = (t >= s)
    c[:, C_DM:C_DM + 128] = np.where((s < 64) & (t >= 64), -1e30, 0.0)
    c[:, C_TB:C_TB + 2048] = -1e-6 * np.arange(2048)[None, :]
    c[:, C_P2:C_P2 + 32] = 2.0 ** -(np.arange(32) + 1.0)
    sel = np.zeros((16, 16, 65))
    for h in range(16):
        sel[h, h, 64] = -8.0
    c[0:16, C_SEL:C_SEL + 16 * 65] = sel.reshape(16, -1)
    p = np.arange(128)
    c[:, C_INV] = np.where((p >= 64) & (p < 96), THETA ** (-2.0 * ((p - 64) % 16) / 32.0), 0.0)
    c[:, C_INV + 1] = np.where(p < 64, THETA ** (-2.0 * (p % 32) / 64.0), 0.0)
    return c


WSPEC = [("x", None, F32), ("positions", None, I32),
         ("ev_w_in", [2, 1024, 1636], F32), ("ev_g_q", [2, 384], F32), ("ev_g_kv", [2, 256], F32),
         ("ev_w_uq", [2, 384, 768], F32), ("ev_w_ukv", [2, 256, 1024], F32), ("ev_w_o", [2, 1024, 1024], F32),
         ("od_w_in", [2, 1024, 3088], F32), ("od_b_f", [2, 16], F32), ("od_w_o", [2, 1024, 1024], F32),
         ("moe_w_grp", [4, 1024, 4], F32), ("moe_b_grp", [4, 4], F32), ("moe_w_sub", [4, 1024, 16], F32),
         ("moe_b_sub", [4, 16], F32), ("moe_w_gate", [4, 16, 1024, 512], F32), ("moe_w_up", [4, 16, 1024, 512], F32),
         ("moe_w_down", [4, 16, 512, 1024], F32), ("ln1_g", [4, 1024], F32), ("ln1_b", [4, 1024], F32),
         ("ln2_g", [4, 1024], F32), ("ln2_b", [4, 1024], F32)]


class Ctx:
    pass


def mm(kb, out, lhsT, rhs, start, stop, r, w):
    kb.op("pe", lambda e: e.matmul(out, lhsT=lhsT, rhs=rhs, start=start, stop=stop), r, w)


def act(kb, out, in_, func, r, w, **kw):
    kb.op("act", lambda e: e.activation(out=out, in_=in_, func=func, **kw), r, w)


def tt(kb, eng, out, in0, in1, op, r, w):
    kb.op(eng, lambda e: e.tensor_tensor(out=out, in0=in0, in1=in1, op=op), r, w)


def ts(kb, eng, out, in0, s1, s2, op0, op1, r, w, accum=None):
    if op1 is None:
        kb.op(eng, lambda e: e.tensor_scalar(out=out, in0=in0, scalar1=s1, scalar2=None, op0=op0), r, w)
    elif accum is None:
        kb.op(eng, lambda e: e.tensor_scalar(out=out, in0=in0, scalar1=s1, scalar2=s2, op0=op0, op1=op1), r, w)
    else:
        kb.op(eng, lambda e: e.tensor_scalar(out=out, in0=in0, scalar1=s1, scalar2=s2, op0=op0, op1=op1, accum_out=accum), r, w)


def stt(kb, out, in0, scalar, in1, op0, op1, r, w):
    kb.op("dve", lambda e: e.scalar_tensor_tensor(out=out, in0=in0, scalar=scalar, in1=in1, op0=op0, op1=op1), r, w)


def cp(kb, eng, out, in_, r, w):
    if eng == "act":
        kb.op("act", lambda e: e.activation(out=out, in_=in_, func=AF.Copy), r, w)
    else:
        kb.op(eng, lambda e: e.tensor_copy(out=out, in_=in_), r, w)


def red(kb, out, in_, op, r, w):
    kb.op("dve", lambda e: e.tensor_reduce(out=out, in_=in_, axis=AX.X, op=op), r, w)


def recip(kb, out, in_, r, w):
    kb.op("dve", lambda e: e.reciprocal(out=out, in_=in_), r, w)


def wload(kb, dst, src, key, r=(), extra_w=()):
    kb.dma("pool", dst, src, r=r, w=[key] + list(extra_w), dsem="w_" + str(key))


def PB(c, i):
    return ("ps", i)


def build(cfg):
    NSEQ = cfg["NSEQ"]
    S = cfg["S"]
    LAYERS = cfg["layers"]
    NT = S // 128
    NB = S // 512
    kb = KB()
    nc = kb.nc
    C = Ctx()
    C.kb = kb
    C.S, C.NT, C.NB = S, NT, NB
    W = {}
    for name, shape, dt in WSPEC:
        if name == "x":
            shape = [NSEQ, S, D]
        if name == "positions":
            shape = [NSEQ, S]
        W[name] = nc.dram_tensor(name, shape, dt, kind="ExternalInput").ap()
    W["consts"] = nc.dram_tensor("consts", [128, CW], F32, kind="ExternalInput").ap()
    OUT = nc.dram_tensor("out", [NSEQ, S, D], F32, kind="ExternalOutput").ap()
    C.W = W
    C.ps = [nc.alloc_psum_tensor("bank%d" % i, [128, 512], F32)[:] for i in range(8)]
    C.X = kb.sb("X", [128, NT, D], F32)
    C.XT = kb.sb("XT", [128, 8, S], BF16)
    C.ident = kb.sb("ident", [128, 128], F32)
    C.identb = kb.sb("identb", [128, 128], BF16)
    C.tri = kb.sb("tri", [128, 128], BF16)
    C.dmask = kb.sb("dmask", [128, 128], F32)
    C.TB = kb.sb("TB", [128, S], BF16)
    C.pow2 = kb.sb("pow2", [128, 32], F32)
    C.inv = kb.sb("inv", [128, 4], F32)
    C.onesf = kb.sb("onesf", [128, 64], F32)
    C.onesb = kb.sb("onesb", [128, 128], BF16)
    cs = W["consts"]
    kb.dma("sp", C.ident, cs[:, C_ID:C_ID + 128], w=["ident"], dsem="c")
    kb.dma("pool", C.identb, cs[:, C_ID:C_ID + 128], w=["identb"], dsem="c2")
    kb.dma("pool", C.tri, cs[:, C_TRI:C_TRI + 128], w=["tri"], dsem="c2")
    kb.dma("sp", C.dmask, cs[:, C_DM:C_DM + 128], w=["dmask"], dsem="c")
    kb.dma("pool", C.TB, cs[:, C_TB:C_TB + S], w=["TB"], dsem="c2")
    kb.dma("sp", C.pow2, cs[:, C_P2:C_P2 + 32], w=["pow2"], dsem="c")
    kb.dma("sp", C.inv, cs[:, C_INV:C_INV + 4], w=["inv"], dsem="c")
    kb.op("dve", lambda e: e.memset(C.onesf, 1.0), w=["onesf"])
    kb.op("dve", lambda e: e.memset(C.onesb, 1.0), w=["onesb"])
    C.base = kb.sb_off
    kb.barrier()
    for seq in range(NSEQ):
        for t in range(NT):
            kb.dma("sp", C.X[:, t, :], W["x"][seq, t * 128:(t + 1) * 128, :], w=[("X", t, 0), ("X", t, 1)], dsem="x%d" % (t % 2))
        for t in range(NT):
            transpose_tile(C, t)
        kb.barrier()
        for li, l in enumerate(LAYERS):
            j = l // 2
            if li % 2 == 0:
                kb.epoch()
            if l % 2 == 1:
                fox_phase(C, j)
            else:
                first = True
                if cfg.get("mla", True):
                    mla_phase(C, j, seq)
                    first = False
                if cfg.get("dsa", True):
                    dsa_phase(C, j, seq, first)
            ln_phase(C, l, 0, None)
            moe_phase(C, l)
            last = (li == len(LAYERS) - 1)
            ln_phase(C, l, 1, (OUT, seq) if last else None)
        kb.barrier()
    kb.op("sp", lambda e: e.nop(), r=["OUT"])
    kb.emit()
    return kb


def transpose_tile(C, t):
    kb = C.kb
    for kc in range(8):
        bank = C.ps[6 + kc // 4]
        kb.op("pe", lambda e, bank=bank, kc=kc: e.transpose(bank[:, (kc % 4) * 128:(kc % 4 + 1) * 128], C.X[:, t, kc * 128:(kc + 1) * 128], C.ident),
              r=[("X", t, kc // 4), "ident"], w=[PB(C, 6 + kc // 4)])
    for hf in range(2):
        cp(kb, "act", C.XT[:, hf * 4:hf * 4 + 4, t * 128:(t + 1) * 128], C.ps[6 + hf].rearrange("p (a b) -> p a b", b=128),
           r=[PB(C, 6 + hf)], w=[("XT", t)])


def ln_phase(C, l, which, outinfo):
    kb = C.kb
    W = C.W
    NT = C.NT
    kb.barrier()
    kb.sb_off = C.base
    LNP = kb.sb("LNP", [128, 2, D], F32)
    BN = kb.sb("BN", [128, 2, 12], F32)
    MV = kb.sb("MV", [128, 2, 4], F32)
    g = W["ln1_g" if which == 0 else "ln2_g"]
    b = W["ln1_b" if which == 0 else "ln2_b"]
    kb.dma("sp", LNP[:, 0, :], g[l:l + 1, :].partition_broadcast(128), w=["LNP"], dsem="lnp")
    kb.dma("sp", LNP[:, 1, :], b[l:l + 1, :].partition_broadcast(128), w=["LNP"], dsem="lnp")
    for t in range(NT):
        p = t % 2
        xk = [("X", t, 0), ("X", t, 1)]
        Xt = C.X[:, t, :]
        for hf in range(2):
            kb.op("dve", lambda e, hf=hf, p=p, t=t: e.bn_stats(out=BN[:, p, hf * 6:hf * 6 + 6], in_=C.X[:, t, hf * 512:(hf + 1) * 512]), r=[("X", t, hf)], w=[("BN", p, hf)])
        kb.op("dve", lambda e, p=p: e.bn_aggr(out=MV[:, p, 0:2], in_=BN[:, p, :]), r=[("BN", p, 0), ("BN", p, 1)], w=[("MV", p)])
        act(kb, MV[:, p, 2:3], MV[:, p, 1:2], AF.Sqrt, r=[("MV", p)], w=[("MV2", p)], bias=1e-5, scale=1.0)
        recip(kb, MV[:, p, 3:4], MV[:, p, 2:3], r=[("MV2", p)], w=[("MV3", p)])
        ts(kb, "dve", Xt, Xt, MV[:, p, 0:1], MV[:, p, 3:4], ALU.subtract, ALU.mult, r=xk + [("MV", p), ("MV3", p)], w=xk)
        tt(kb, "pool", Xt, Xt, LNP[:, 0, :], ALU.mult, r=xk + ["LNP"], w=xk)
        tt(kb, "pool", Xt, Xt, LNP[:, 1, :], ALU.add, r=xk + ["LNP"], w=xk)
        if outinfo is not None:
            OUT, seq = outinfo
            kb.dma("sp", OUT[seq, t * 128:(t + 1) * 128, :], Xt, r=xk, w=["OUT"], dsem="out")
        else:
            transpose_tile(C, t)
            if which == 0:
                ts(kb, "pool", Xt, Xt, ALPHA, 1.0, ALU.mult, ALU.mult, r=xk, w=xk)
    kb.barrier()


def moe_phase(C, l):
    kb = C.kb
    W = C.W
    NT, NB = C.NT, C.NB
    kb.sb_off = C.base
    Wg = [kb.sb("Wg%d" % i, [128, 8, 512], BF16) for i in range(2)]
    Wu = [kb.sb("Wu%d" % i, [128, 8, 512], BF16) for i in range(2)]
    Wd = [kb.sb("Wd%d" % i, [128, 4, 1024], BF16) for i in range(2)]
    Wr = kb.sb("Wr", [128, 8, 20], BF16)
    brow = kb.sb("brow", [128, 20], F32)
    L = kb.sb("L", [128, NT, 20], F32)
    G = kb.sb("G", [128, NT, 16], F32)
    T1 = kb.sb("T1", [128, NT, 16], F32)
    T2 = kb.sb("T2", [128, NT, 16], F32)
    T3 = kb.sb("T3", [128, NT, 16], F32)
    SM = kb.sb("SM", [128, 12, NT], F32)
    hT = [kb.sb("hT%d" % i, [128, 4, 512], BF16) for i in range(2)]
    sg = [kb.sb("sg%d" % i, [128, 512], F32) for i in range(2)]

    def load_expert(e):
        i = e % 2
        wload(kb, Wg[i], W["moe_w_gate"][l, e].rearrange("(kc p) f -> p kc f", p=128), ("Wg", i))
        wload(kb, Wu[i], W["moe_w_up"][l, e].rearrange("(kc p) f -> p kc f", p=128), ("Wu", i))
        wload(kb, Wd[i], W["moe_w_down"][l, e].rearrange("(kc p) f -> p kc f", p=128), ("Wd", i))

    load_expert(0)
    wload(kb, Wr[:, :, 0:4], W["moe_w_grp"][l].rearrange("(kc p) f -> p kc f", p=128), "Wr")
    wload(kb, Wr[:, :, 4:20], W["moe_w_sub"][l].rearrange("(kc p) f -> p kc f", p=128), "Wr")
    kb.dma("sp", brow[:, 0:4], W["moe_b_grp"][l:l + 1, :].partition_broadcast(128), w=["brow"], dsem="brow")
    kb.dma("sp", brow[:, 4:20], W["moe_b_sub"][l:l + 1, :].partition_broadcast(128), w=["brow"], dsem="brow")
    for t in range(NT):
        bank = C.ps[6 + t % 2]
        for kc in range(8):
            mm(kb, bank[:, 0:20], C.XT[:, kc, t * 128:(t + 1) * 128], Wr[:, kc, :], kc == 0, kc == 7, r=[("XT", t), "Wr"], w=[PB(C, 6 + t % 2)])
        tt(kb, "dve", L[:, t, :], bank[:, 0:20], brow, ALU.add, r=[PB(C, 6 + t % 2), "brow"], w=["L"])
    lg = L[:, :, 0:4]
    ls = L[:, :, 4:20]
    m = SM[:, 0, :]
    se = SM[:, 1, :]
    ptop = SM[:, 2, :]
    v1 = SM[:, 3, :]
    v2 = SM[:, 4, :]
    dd = SM[:, 5, :]
    w1 = SM[:, 6, :]
    w1p = SM[:, 7, :]
    w2p = SM[:, 8, :]
    oh = T1[:, :, 0:4]
    sh = T1[:, :, 4:8]
    pen = T1[:, :, 8:12]

    def bc(ap, n):
        return ap.unsqueeze(2).broadcast_to([128, NT, n])

    red(kb, m, lg, ALU.max, r=["L"], w=["m"])
    tt(kb, "dve", oh, lg, bc(m, 4), ALU.is_equal, r=["L", "m"], w=["oh"])
    tt(kb, "dve", sh, lg, bc(m, 4), ALU.subtract, r=["L", "m"], w=["sh"])
    act(kb, sh, sh, AF.Exp, r=["sh"], w=["sh"])
    red(kb, se, sh, ALU.add, r=["sh"], w=["se"])
    recip(kb, ptop, se, r=["se"], w=["ptop"])
    ts(kb, "dve", pen, oh, 1.0, 1e30, ALU.subtract, ALU.mult, r=["oh"], w=["pen"])
    tt(kb, "dve", T2.rearrange("p t (a b) -> p t a b", b=4), ls.rearrange("p t (a b) -> p t a b", b=4),
       pen.unsqueeze(3).broadcast_to([128, NT, 4, 4]), ALU.add, r=["L", "pen"], w=["T2"])
    red(kb, v1, T2, ALU.max, r=["T2"], w=["v1"])
    tt(kb, "dve", T3, T2, bc(v1, 16), ALU.is_equal, r=["T2", "v1"], w=["T3"])
    stt(kb, T2, T3, -1e30, T2, ALU.mult, ALU.add, r=["T3", "T2"], w=["T2"])
    red(kb, v2, T2, ALU.max, r=["T2"], w=["v2"])
    tt(kb, "dve", T2, T2, bc(v2, 16), ALU.is_equal, r=["T2", "v2"], w=["T2"])
    tt(kb, "dve", dd, v2, v1, ALU.subtract, r=["v1", "v2"], w=["dd"])
    act(kb, dd, dd, AF.Exp, r=["dd"], w=["dd"])
    ts(kb, "dve", dd, dd, 1.0, None, ALU.add, None, r=["dd"], w=["dd"])
    recip(kb, w1, dd, r=["dd"], w=["w1"])
    tt(kb, "dve", w1p, w1, ptop, ALU.mult, r=["w1", "ptop"], w=["w1p"])
    tt(kb, "dve", w2p, ptop, w1p, ALU.subtract, r=["w1p", "ptop"], w=["w2p"])
    tt(kb, "dve", G, T3, bc(w1p, 16), ALU.mult, r=["T3", "w1p"], w=["G"])
    tt(kb, "dve", T2, T2, bc(w2p, 16), ALU.mult, r=["T2", "w2p"], w=["T2"])
    tt(kb, "dve", G, G, T2, ALU.add, r=["G", "T2"], w=["G"])
    cnt = 0
    for e in range(16):
        i = e % 2
        if e + 1 < 16:
            load_expert(e + 1)
        for b in range(NB):
            hb = cnt % 2
            cnt += 1
            blk = slice(b * 512, (b + 1) * 512)
            xr = [("XT", b * 4 + q) for q in range(4)]
            for fc in range(4):
                pg = fc % 2
                pu = 2 + fc % 2
                for kc in range(8):
                    mm(kb, C.ps[pg], Wg[i][:, kc, fc * 128:(fc + 1) * 128], C.XT[:, kc, blk], kc == 0, kc == 7, r=xr + [("Wg", i)], w=[PB(C, pg)])
                for kc in range(8):
                    mm(kb, C.ps[pu], Wu[i][:, kc, fc * 128:(fc + 1) * 128], C.XT[:, kc, blk], kc == 0, kc == 7, r=xr + [("Wu", i)], w=[PB(C, pu)])
                act(kb, sg[fc % 2], C.ps[pg], AF.Silu, r=[PB(C, pg)], w=[("sg", fc % 2)])
                tt(kb, "dve", hT[hb][:, fc, :], sg[fc % 2], C.ps[pu], ALU.mult, r=[("sg", fc % 2), PB(C, pu)], w=[("hT", hb, fc)])
            for q in range(4):
                t = b * 4 + q
                for hf in range(2):
                    py = 4 + hf
                    for fc in range(4):
                        mm(kb, C.ps[py], hT[hb][:, fc, q * 128:(q + 1) * 128], Wd[i][:, fc, hf * 512:(hf + 1) * 512], fc == 0, fc == 3,
                           r=[("hT", hb, fc), ("Wd", i)], w=[PB(C, py)])
                    Xh = C.X[:, t, hf * 512:(hf + 1) * 512]
                    stt(kb, Xh, C.ps[py], G[:, t, e:e + 1], Xh, ALU.mult, ALU.add, r=[PB(C, py), "G", ("X", t, hf)], w=[("X", t, hf)])


def attn_norm_a(C, OTb, nrows_key):
    kb = C.kb
    recip(kb, C.RR[64:65, :], OTb[64:65, :], r=[nrows_key], w=["RR"])
    cp(kb, "dve", C.RRb[64:65, 0, :], C.RR[64:65, :], r=["RR"], w=["RRh"])
    tt(kb, "dve", C.RRb[64:65, 1, :], C.RR[64:65, :], C.RRb[64:65, 0, :], ALU.subtract, r=["RR", "RRh"], w=["RRl"])


def attn_norm_b(C, OTb, nrows_key, out_ap, shape3=None):
    kb = C.kb
    mm(kb, C.ps[6][0:64, :], C.onesb[64:65, 0:64], C.RRb[64:65, 0, :], True, False, r=["RRh", "onesb"], w=[PB(C, 6)])
    mm(kb, C.ps[6][0:64, :], C.onesb[64:65, 0:64], C.RRb[64:65, 1, :], False, True, r=["RRl", "onesb"], w=[PB(C, 6)])
    cp(kb, "act", C.BCs, C.ps[6][0:64, :], r=[PB(C, 6)], w=["BCs"])
    if shape3 is None:
        tt(kb, "dve", out_ap, OTb[0:64, :], C.BCs, ALU.mult, r=[nrows_key, "BCs"], w=["OTs"])
    else:
        tt(kb, "dve", out_ap, OTb[0:64, :].rearrange("p (a b) -> p a b", b=shape3), C.BCs.rearrange("p (a b) -> p a b", b=shape3),
           ALU.mult, r=[nrows_key, "BCs"], w=["OTs"])


LOOK = 3


def run_pipelined(steps):
    pend = []
    for n in range(min(LOOK, len(steps))):
        steps[n]["st"]()
    for n, sp in enumerate(steps):
        if n + LOOK < len(steps):
            steps[n + LOOK]["st"]()
        sp["ex"]()
        sp["pv"]()
        newp = []
        for (cnt_, f) in pend:
            if cnt_ <= 0:
                f()
            else:
                newp.append((cnt_ - 1, f))
        pend = newp
        if "na" in sp:
            sp["na"]()
            pend.append((2, sp["nb"]))
    for (_, f) in pend:
        f()


def attn_block(C, b, nh, KT, QT, V, Krows, scale, biasf, diag, OTs):
    kb = C.kb
    nkt = 4 * b + 4
    steps = []
    for hh in range(nh):
        otb = 4 + hh % 2
        OTb = C.ps[otb]
        for kt in range(nkt):
            r_ = kt - 4 * b
            q0 = max(r_, 0) * 128
            nq = 512 - q0
            sb_ = C.cnt % 4
            pi = C.cnt % 4
            C.cnt += 1

            def st(hh=hh, kt=kt, q0=q0, nq=nq, sb_=sb_):
                mm(kb, C.ps[sb_][:, 0:nq], KT[0:Krows, hh, kt * 128:(kt + 1) * 128], QT[0:Krows, hh, q0:512], True, True,
                   r=[("KT", kt // 4), "QT"], w=[PB(C, sb_)])

            def ex(hh=hh, kt=kt, r_=r_, nq=nq, sb_=sb_, pi=pi):
                kw = dict(scale=scale)
                rk = [PB(C, sb_)]
                if biasf is not None:
                    kw["bias"] = biasf(kt, hh)
                    rk.append("CB")
                act(kb, C.PT[pi][:, 0:nq], C.ps[sb_][:, 0:nq], AF.Exp, r=rk, w=[("PT", pi)], **kw)
                if r_ >= 0:
                    if diag == "tri":
                        tt(kb, "pool", C.PT[pi][:, 0:128], C.PT[pi][:, 0:128], C.tri, ALU.mult, r=[("PT", pi), "tri"], w=[("PT", pi)])
                    else:
                        kb.op("pool", lambda e, pi=pi: e.memset(C.PT[pi][64:128, 0:64], 0.0), r=[("PT", pi)], w=[("PT", pi)])

            def pv(hh=hh, kt=kt, q0=q0, nq=nq, pi=pi, OTb=OTb, otb=otb):
                mm(kb, OTb[0:65, q0:512], V[:, kt, hh, :], C.PT[pi][:, 0:nq], kt == 0, kt == nkt - 1, r=[("V", kt // 4), ("PT", pi)], w=[PB(C, otb)])

            sp = dict(st=st, ex=ex, pv=pv)
            if kt == nkt - 1:
                sp["na"] = lambda OTb=OTb, otb=otb: attn_norm_a(C, OTb, PB(C, otb))
                sp["nb"] = lambda OTb=OTb, otb=otb, hh=hh: attn_norm_b(C, OTb, PB(C, otb), OTs[:, hh, :])
            steps.append(sp)
    run_pipelined(steps)


def outproj_block(C, b, nh, OTs, Wo, first):
    kb = C.kb
    for q in range(4):
        t = b * 4 + q
        for hf in range(2):
            bi = (q * 2 + hf) % 4
            bank = C.ps[bi]
            for hh in range(nh):
                mm(kb, bank, OTs[:, hh, q * 128:(q + 1) * 128], Wo[:, hh, hf * 512:(hf + 1) * 512], hh == 0, hh == nh - 1, r=["OTs", "Wo"], w=[PB(C, bi)])
            Xh = C.X[:, t, hf * 512:(hf + 1) * 512]
            stt(kb, Xh, Xh, ALPHA if first else 1.0, bank, ALU.mult, ALU.add, r=[PB(C, bi), ("X", t, hf)], w=[("X", t, hf)])


def alloc_attn_common(C):
    kb = C.kb
    C.PT = [kb.sb("PT%d" % i, [128, 512], BF16) for i in range(4)]
    C.RRb = kb.sb("RRb", [128, 2, 512], BF16)
    C.RR = kb.sb("RR", [128, 512], F32)
    C.BCs = C.RR[0:64]
    C.cnt = 0


def fox_phase(C, j):
    kb = C.kb
    W = C.W
    S, NT, NB = C.S, C.NT, C.NB
    kb.barrier()
    kb.sb_off = C.base
    alloc_attn_common(C)
    win = W["od_w_in"][j]
    Wq = kb.sb("fWq", [128, 8, 4, 65], BF16)
    Wk = kb.sb("fWk", [128, 8, 4, 64], BF16)
    Wv = kb.sb("fWv", [128, 8, 256], BF16)
    Wf = kb.sb("fWf", [128, 8, 16], BF16)
    Wo = kb.sb("fWo", [64, 4, 1024], BF16)
    bf = kb.sb("fbf", [16, 2], F32)
    NL = kb.sb("fNL", [16, S], F32)
    CP = kb.sb("fCP", [16, S], F32)
    CPb = kb.sb("fCPb", [16, S], BF16)
    ones16 = kb.sb("fones", [16, 512], F32)
    C.SEL = kb.sb("SEL", [16, 16, 65], BF16)
    kb.dma("pool", C.SEL, W["consts"][0:16, C_SEL:C_SEL + 16 * 65].rearrange("p (a b) -> p a b", b=65), w=["SEL"], dsem="c2")
    CB = kb.sb("fCB", [128, NT, 16], F32)
    KT = kb.sb("fKT", [65, 4, S], BF16)
    V = kb.sb("fV", [128, NT, 4, 65], BF16)
    QT = kb.sb("fQT", [65, 4, 512], BF16)
    OTs = kb.sb("fOTs", [64, 4, 512], BF16)
    kb.op("pool", lambda e: e.memset(Wq, 0.0), w=["Wq"])
    kb.op("pool", lambda e: e.memset(KT[64:65], 1.0), w=[("KT", b) for b in range(NB)])
    kb.op("pool", lambda e: e.memset(V, 1.0), w=[("V", b) for b in range(NB)])
    kb.op("pool", lambda e: e.memset(ones16, 1.0), w=["ones16"])
    wload(kb, Wf, win[:, 3072:3088].rearrange("(kc p) f -> p kc f", p=128), "Wf")
    kb.dma("sp", bf[:, 0:1], W["od_b_f"][j:j + 1, :].rearrange("a h -> h a"), w=["bf"], dsem="bf")
    ts(kb, "dve", bf[:, 1:2], bf[:, 0:1], -1.0, None, ALU.mult, None, r=["bf"], w=["nbf"])
    for b in range(NB):
        blk = slice(b * 512, (b + 1) * 512)
        for kc in range(8):
            mm(kb, C.ps[0][0:16, :], Wf[:, kc, :], C.XT[:, kc, blk], kc == 0, kc == 7, r=[("XT", b * 4 + q) for q in range(4)] + ["Wf"], w=[PB(C, 0)])
        act(kb, NL[:, blk], C.ps[0][0:16, :], AF.Exp, r=[PB(C, 0), "nbf"], w=["NL"], scale=-1.0, bias=bf[:, 1:2])
        act(kb, NL[:, blk], NL[:, blk], AF.Ln, r=["NL"], w=["NL"], bias=1.0, scale=1.0)
        init = 0.0 if b == 0 else CP[:, b * 512 - 1:b * 512]
        kb.op("dve", lambda e, blk=blk, init=init: e.tensor_tensor_scan(out=CP[:, blk], data0=ones16, data1=NL[:, blk], initial=init, op0=ALU.mult, op1=ALU.add),
              r=["NL", "ones16", "CP"], w=["CP"])
    cp(kb, "act", CPb, CP, r=["CP"], w=["CPb"])
    for kt in range(NT):
        kb.op("pe", lambda e, kt=kt: e.transpose(C.ps[1][:, 0:16], CP[:, kt * 128:(kt + 1) * 128], C.ident[0:16, 0:16]), r=["CP", "ident"], w=[PB(C, 1)])
        cp(kb, "dve", CB[:, kt, :], C.ps[1][:, 0:16], r=[PB(C, 1)], w=["CB"])
    for g in range(4):
        for hh in range(4):
            h = g * 4 + hh
            wload(kb, Wq[:, :, hh, 0:64], win[:, h * 64:(h + 1) * 64].rearrange("(kc p) f -> p kc f", p=128), "Wq")
            wload(kb, Wk[:, :, hh, :], win[:, 1024 + h * 64:1024 + (h + 1) * 64].rearrange("(kc p) f -> p kc f", p=128), "Wk")
        wload(kb, Wv, win[:, 2048 + g * 256:2048 + (g + 1) * 256].rearrange("(kc p) f -> p kc f", p=128), "Wv")
        wload(kb, Wo, W["od_w_o"][j][g * 256:(g + 1) * 256, :].rearrange("(h d) n -> d h n", d=64), "Wo")
        for b in range(NB):
            blk = slice(b * 512, (b + 1) * 512)
            xr = [("XT", b * 4 + q) for q in range(4)]
            for hh in range(4):
                h = g * 4 + hh
                pb = hh % 2
                for kc in range(8):
                    mm(kb, C.ps[pb][0:65, :], Wq[:, kc, hh, :], C.XT[:, kc, blk], kc == 0, False, r=xr + ["Wq"], w=[PB(C, pb)])
                mm(kb, C.ps[pb][0:65, :], C.SEL[0:16, h, :], CPb[:, blk], False, True, r=["SEL", "CPb"], w=[PB(C, pb)])
                cp(kb, "act", QT[:, hh, :], C.ps[pb][0:65, :], r=[PB(C, pb)], w=["QT"])
            for hh in range(4):
                pb = hh % 2
                for kc in range(8):
                    mm(kb, C.ps[pb][0:64, :], Wk[:, kc, hh, :], C.XT[:, kc, blk], kc == 0, kc == 7, r=xr + ["Wk"], w=[PB(C, pb)])
                cp(kb, "dve", KT[0:64, hh, blk], C.ps[pb][0:64, :], r=[PB(C, pb)], w=[("KT", b)])
            for q in range(4):
                t = b * 4 + q
                pb = q % 2
                for kc in range(8):
                    mm(kb, C.ps[pb][:, 0:256], C.XT[:, kc, t * 128:(t + 1) * 128], Wv[:, kc, :], kc == 0, kc == 7, r=[("XT", t), "Wv"], w=[PB(C, pb)])
                cp(kb, "act", V[:, t, :, 0:64], C.ps[pb][:, 0:256].rearrange("p (a b) -> p a b", b=64), r=[PB(C, pb)], w=[("V", b)])
            attn_block(C, b, 4, KT, QT, V, 65, 0.125, lambda kt, hh, g=g: CB[:, kt, g * 4 + hh:g * 4 + hh + 1], "tri", OTs)
            outproj_block(C, b, 4, OTs, Wo, g == 0)
    kb.barrier()


def rope_tables(C, seq, b, tabs):
    kb = C.kb
    W = C.W
    kA, kF = C.kANG, C.kKF
    kb.dma("sp", C.POSI, W["positions"][seq:seq + 1, b * 512:(b + 1) * 512].partition_broadcast(128), w=["POSI"], dsem="pos")
    cp(kb, "dve", C.POSF, C.POSI, r=["POSI"], w=["POSF"])
    for (ic, nr, COS, SIN) in tabs:
        for which, dst in ((0, SIN), (1, COS)):
            ANG = C.ANG[0:nr]
            if which == 0:
                ts(kb, "dve", ANG, C.POSF[0:nr], C.inv[0:nr, ic:ic + 1], None, ALU.mult, None, r=["POSF", "inv", "ROPE"], w=[kA])
            else:
                ts(kb, "dve", ANG, C.POSF[0:nr], C.inv[0:nr, ic:ic + 1], math.pi / 2, ALU.mult, ALU.add, r=["POSF", "inv", "ROPE"], w=[kA])
            ts(kb, "dve", C.KI[0:nr], ANG, 1.0 / TWO_PI, None, ALU.mult, None, r=[kA], w=["POSI"])
            cp(kb, "dve", C.KF[0:nr], C.KI[0:nr], r=["POSI"], w=[kF])
            stt(kb, ANG, C.KF[0:nr], -TWO_PI, ANG, ALU.mult, ALU.add, r=[kF, kA], w=[kA])
            ts(kb, "dve", ANG, ANG, -3.14159, 3.14159, ALU.max, ALU.min, r=[kA], w=[kA])
            act(kb, dst, ANG, AF.Sin, r=[kA], w=["ROPE"])


def alloc_rope(C, TA, TBf):
    kb = C.kb
    C.POSI = kb.sb("POSI", [128, 512], I32)
    C.POSF = kb.sb("POSF", [128, 512], F32)
    C.KI = C.POSI
    C.KF = TA
    C.ANG = TBf
    C.kKF = "TA"
    C.kANG = "TBf"


def load_rot(kb, dst_plain, dst_rot, src_cols_fn, d, key):
    h = d // 2
    wload(kb, dst_plain, src_cols_fn(0, d), key)
    wload(kb, dst_rot[:, :, 0:h], src_cols_fn(h, d), key)
    wload(kb, dst_rot[:, :, h:d], src_cols_fn(0, h), key)
    ts(kb, "pool", dst_rot[:, :, 0:h], dst_rot[:, :, 0:h], -1.0, 1.0, ALU.mult, ALU.mult, r=[key], w=[key])


def mla_phase(C, j, seq):
    kb = C.kb
    W = C.W
    S, NT, NB = C.S, C.NT, C.NB
    kb.barrier()
    kb.sb_off = C.base
    alloc_attn_common(C)
    win = W["ev_w_in"][j]
    Wcq = kb.sb("mWcq", [128, 8, 384], BF16)
    Wckv = kb.sb("mWckv", [128, 8, 256], BF16)
    Wkr = kb.sb("mWkr", [128, 8, 2, 96], BF16)
    Wuq = kb.sb("mWuq", [128, 3, 8, 96], BF16)
    Wuqr = kb.sb("mWuqr", [128, 3, 8, 96], BF16)
    Wukk = kb.sb("mWukk", [128, 2, 8, 64], BF16)
    Wukv = kb.sb("mWukv", [128, 2, 8, 64], BF16)
    Wo = kb.sb("mWo", [64, 4, 1024], BF16)
    gq = kb.sb("mgq", [128, 3], F32)
    gkv = kb.sb("mgkv", [128, 2], F32)
    CQ = kb.sb("mCQ", [128, 3, 512], F32)
    SQ = [kb.sb("mSQ%d" % i, [128, 512], BF16) for i in range(2)]
    RBC = kb.sb("mRBC", [128, 512], F32)
    CQN = kb.sb("mCQN", [128, 3, 512], BF16)
    CKVN = kb.sb("mCKVN", [128, 2, 512], BF16)
    COS = kb.sb("mCOS", [96, 512], F32)
    SIN = kb.sb("mSIN", [96, 512], F32)
    TAx = kb.sb("mTA", [128, 512], F32)
    TBx = kb.sb("mTB", [128, 512], F32)
    alloc_rope(C, TAx, TBx)
    TA = TAx[0:96]
    TBf = TBx[0:96]
    KT = kb.sb("mKT", [96, 4, S], BF16)
    V = kb.sb("mV", [128, NT, 4, 65], BF16)
    QT = kb.sb("mQT", [96, 4, 512], BF16)
    OTs = kb.sb("mOTs", [64, 4, 512], BF16)
    kb.op("pool", lambda e: e.memset(Wkr, 0.0), w=["Wkr"])
    kb.op("pool", lambda e: e.memset(Wuqr, 0.0), w=["Wuqr"])
    kb.op("pool", lambda e: e.memset(V, 1.0), w=[("V", b) for b in range(NB)])

    def cols(c0, c1):
        return win[:, c0:c1].rearrange("(kc p) f -> p kc f", p=128)

    wload(kb, Wcq, cols(0, 384), "Wcq")
    wload(kb, Wckv, cols(384, 640), "Wckv")
    load_rot(kb, Wkr[:, :, 0, 64:96], Wkr[:, :, 1, 64:96], lambda a, b_: cols(640 + a, 640 + b_), 32, "Wkr")
    uq = W["ev_w_uq"][j]
    for h in range(8):
        wload(kb, Wuq[:, :, h, :], uq[:, h * 96:(h + 1) * 96].rearrange("(kc p) f -> p kc f", p=128), "Wuq")
        wload(kb, Wuqr[:, :, h, 64:80], uq[:, h * 96 + 80:h * 96 + 96].rearrange("(kc p) f -> p kc f", p=128), "Wuqr")
        wload(kb, Wuqr[:, :, h, 80:96], uq[:, h * 96 + 64:h * 96 + 80].rearrange("(kc p) f -> p kc f", p=128), "Wuqr")
    ts(kb, "pool", Wuqr[:, :, :, 64:80], Wuqr[:, :, :, 64:80], -1.0, 1.0, ALU.mult, ALU.mult, r=["Wuqr"], w=["Wuqr"])
    ukv = W["ev_w_ukv"][j]
    for h in range(8):
        wload(kb, Wukk[:, :, h, :], ukv[:, h * 128:h * 128 + 64].rearrange("(kc p) f -> p kc f", p=128), "Wukk")
        wload(kb, Wukv[:, :, h, :], ukv[:, h * 128 + 64:h * 128 + 128].rearrange("(kc p) f -> p kc f", p=128), "Wukv")
    for kc in range(3):
        kb.dma("sp", gq[:, kc:kc + 1], W["ev_g_q"][j:j + 1, kc * 128:(kc + 1) * 128].rearrange("a p -> p a"), w=["gq"], dsem="gq")
    for kc in range(2):
        kb.dma("sp", gkv[:, kc:kc + 1], W["ev_g_kv"][j:j + 1, kc * 128:(kc + 1) * 128].rearrange("a p -> p a"), w=["gkv"], dsem="gq")
    scale = 96.0 ** -0.5
    for g in range(2):
        wload(kb, Wo, W["ev_w_o"][j][g * 256:(g + 1) * 256, :].rearrange("(h d) n -> d h n", d=64), "Wo")
        for b in range(NB):
            blk = slice(b * 512, (b + 1) * 512)
            xr = [("XT", b * 4 + q) for q in range(4)]
            kb.barrier()
            rope_tables(C, seq, b, [(0, 96, COS, SIN)])
            for (Wc, nch, gcol, OUTN, nfeat, eps) in ((Wcq, 3, gq, CQN, 384.0, 1e-6), (Wckv, 2, gkv, CKVN, 256.0, 1e-6)):
                for c in range(nch):
                    pb = c % 2
                    for kc in range(8):
                        mm(kb, C.ps[pb], Wc[:, kc, c * 128:(c + 1) * 128], C.XT[:, kc, blk], kc == 0, kc == 7, r=xr + ["Wcq", "Wckv"], w=[PB(C, pb)])
                    cp(kb, "act", CQ[:, c, :], C.ps[pb], r=[PB(C, pb)], w=[("CQ", c)])
                    act(kb, SQ[c % 2], C.ps[pb], AF.Square, r=[PB(C, pb)], w=[("SQ", c % 2)])
                    mm(kb, C.ps[6], C.onesb, SQ[c % 2], c == 0, c == nch - 1, r=[("SQ", c % 2), "onesb"], w=[PB(C, 6)])
                act(kb, RBC, C.ps[6], AF.Sqrt, r=[PB(C, 6)], w=["RBC"], scale=1.0 / nfeat, bias=eps)
                recip(kb, RBC, RBC, r=["RBC"], w=["RBC"])
                for c in range(nch):
                    stt(kb, OUTN[:, c, :], CQ[:, c, :], gcol[:, c:c + 1], RBC, ALU.mult, ALU.mult, r=[("CQ", c), "RBC", "gq", "gkv"], w=["OUTN"])
            for kc in range(8):
                mm(kb, C.ps[0][0:96, :], Wkr[:, kc, 0, :], C.XT[:, kc, blk], kc == 0, kc == 7, r=xr + ["Wkr"], w=[PB(C, 0)])
            for kc in range(8):
                mm(kb, C.ps[1][0:96, :], Wkr[:, kc, 1, :], C.XT[:, kc, blk], kc == 0, kc == 7, r=xr + ["Wkr"], w=[PB(C, 1)])
            tt(kb, "dve", TA[64:96], C.ps[0][64:96, :], COS[64:96], ALU.mult, r=[PB(C, 0), "ROPE"], w=["TA"])
            tt(kb, "dve", TBf[64:96], C.ps[1][64:96, :], SIN[64:96], ALU.mult, r=[PB(C, 1), "ROPE"], w=["TBf"])
            for hh in range(4):
                tt(kb, "pool", KT[64:96, hh, blk], TA[64:96], TBf[64:96], ALU.add, r=["TA", "TBf"], w=[("KT", b)])
            for hh in range(4):
                h = g * 4 + hh
                pb = hh % 2
                for kc in range(2):
                    mm(kb, C.ps[pb][0:64, :], Wukk[:, kc, h, :], CKVN[:, kc, :], kc == 0, kc == 1, r=["OUTN", "Wukk"], w=[PB(C, pb)])
                cp(kb, "act", KT[0:64, hh, blk], C.ps[pb][0:64, :], r=[PB(C, pb)], w=[("KT", b)])
            for q in range(4):
                t = b * 4 + q
                pb = q % 2
                for kc in range(2):
                    mm(kb, C.ps[pb][:, 0:256], CKVN[:, kc, q * 128:(q + 1) * 128], Wukv[:, kc, g * 4:(g + 1) * 4, :], kc == 0, kc == 1, r=["OUTN", "Wukv"], w=[PB(C, pb)])
                cp(kb, "act", V[:, t, :, 0:64], C.ps[pb][:, 0:256].rearrange("p (a b) -> p a b", b=64), r=[PB(C, pb)], w=[("V", b)])
            for hh in range(4):
                h = g * 4 + hh
                for kc in range(3):
                    mm(kb, C.ps[0][0:96, :], Wuq[:, kc, h, :], CQN[:, kc, :], kc == 0, kc == 2, r=["OUTN", "Wuq"], w=[PB(C, 0)])
                for kc in range(3):
                    mm(kb, C.ps[1][0:96, :], Wuqr[:, kc, h, :], CQN[:, kc, :], kc == 0, kc == 2, r=["OUTN", "Wuqr"], w=[PB(C, 1)])
                tt(kb, "dve", TA, C.ps[0][0:96, :], COS, ALU.mult, r=[PB(C, 0), "ROPE"], w=["TA"])
                tt(kb, "dve", TBf, C.ps[1][0:96, :], SIN, ALU.mult, r=[PB(C, 1), "ROPE"], w=["TBf"])
                tt(kb, "pool", QT[:, hh, :], TA, TBf, ALU.add, r=["TA", "TBf"], w=["QT"])
            attn_block(C, b, 4, KT, QT, V, 96, scale, None, "chunk", OTs)
            outproj_block(C, b, 4, OTs, Wo, g == 0)
    kb.barrier()


def dsa_phase(C, j, seq, first):
    kb = C.kb
    W = C.W
    S, NT, NB = C.S, C.NT, C.NB
    kb.barrier()
    kb.sb_off = C.base
    alloc_attn_common(C)
    win = W["ev_w_in"][j]
    Wqb = kb.sb("dWqb", [128, 8, 2, 512], BF16)
    Wkb = kb.sb("dWkb", [128, 8, 2, 64], BF16)
    Wvb = kb.sb("dWvb", [128, 8, 64], BF16)
    Wqi = kb.sb("dWqi", [128, 8, 2, 256], BF16)
    Wki = kb.sb("dWki", [128, 8, 2, 64], BF16)
    Wwi = kb.sb("dWwi", [128, 8, 4], BF16)
    Wo = kb.sb("dWo", [64, 8, 1024], BF16)
    KTb = kb.sb("dKTb", [64, S], BF16)
    Vb = kb.sb("dVb", [128, NT, 65], BF16)
    KTi = kb.sb("dKTi", [64, S], BF16)
    QTb = kb.sb("dQTb", [64, 8, 512], BF16)
    QTi = kb.sb("dQTi", [64, 4, 512], BF16)
    WI = kb.sb("dWI", [128, 4, 4], F32)
    OTs = kb.sb("dOTs", [64, 8, 512], BF16)
    MT = kb.sb("dMT", [128, NT, 128], BF16)
    TAU = kb.sb("dTAU", [128, 8], F32)
    STEPS = kb.sb("dSTEPS", [128, 32], F32)
    offA = kb.sb_off
    COS = kb.sb("dCOS", [64, 512], F32)
    SIN = kb.sb("dSIN", [64, 512], F32)
    TAx = kb.sb("dTA", [128, 512], F32)
    TBx = kb.sb("dTB", [128, 512], F32)
    alloc_rope(C, TAx, TBx)
    TA = TAx[0:64]
    TBf = TBx[0:64]
    endA = kb.sb_off
    kb.sb_off = offA
    SC = kb.sb("dSC", [128, S], F32)
    RL = kb.sb("dRL", [128, 512], F32)
    MASK = kb.sb("dMASK", [128, S], BF16)
    kb.sb_off = max(endA, kb.sb_off)
    psb7 = C.ps[7].bitcast(BF16)

    def cols(c0, c1):
        return win[:, c0:c1].rearrange("(kc p) f -> p kc f", p=128)

    kb.op("pool", lambda e: e.memset(Vb, 1.0), w=[("V", b) for b in range(NB)])
    for h in range(8):
        load_rot(kb, Wqb[:, :, 0, h * 64:(h + 1) * 64], Wqb[:, :, 1, h * 64:(h + 1) * 64], lambda a, b_, h=h: cols(672 + h * 64 + a, 672 + h * 64 + b_), 64, "Wqb")
    load_rot(kb, Wkb[:, :, 0, :], Wkb[:, :, 1, :], lambda a, b_: cols(1184 + a, 1184 + b_), 64, "Wkb")
    wload(kb, Wvb, cols(1248, 1312), "Wvb")
    for h in range(4):
        load_rot(kb, Wqi[:, :, 0, h * 64:(h + 1) * 64], Wqi[:, :, 1, h * 64:(h + 1) * 64], lambda a, b_, h=h: cols(1312 + h * 64 + a, 1312 + h * 64 + b_), 64, "Wqi")
    load_rot(kb, Wki[:, :, 0, :], Wki[:, :, 1, :], lambda a, b_: cols(1568 + a, 1568 + b_), 64, "Wki")
    wload(kb, Wwi, cols(1632, 1636), "Wwi")
    wload(kb, Wo, W["ev_w_o"][j][512:1024, :].rearrange("(h d) n -> d h n", d=64), "Wo")

    def roped(Wt, c0, dst, key_w, wkeys):
        for kc in range(8):
            mm(kb, C.ps[0][0:64, :], Wt[:, kc, 0, c0:c0 + 64], C.XT[:, kc, roped.blk], kc == 0, kc == 7, r=roped.xr + [key_w], w=[PB(C, 0)])
        for kc in range(8):
            mm(kb, C.ps[1][0:64, :], Wt[:, kc, 1, c0:c0 + 64], C.XT[:, kc, roped.blk], kc == 0, kc == 7, r=roped.xr + [key_w], w=[PB(C, 1)])
        tt(kb, "dve", TA, C.ps[0][0:64, :], COS, ALU.mult, r=[PB(C, 0), "ROPE"], w=["TA"])
        tt(kb, "dve", TBf, C.ps[1][0:64, :], SIN, ALU.mult, r=[PB(C, 1), "ROPE"], w=["TBf"])
        tt(kb, "pool", dst, TA, TBf, ALU.add, r=["TA", "TBf"], w=wkeys)

    for b in range(NB):
        blk = slice(b * 512, (b + 1) * 512)
        roped.blk = blk
        roped.xr = [("XT", b * 4 + q) for q in range(4)]
        kb.barrier()
        rope_tables(C, seq, b, [(1, 64, COS, SIN)])
        for h in range(8):
            roped(Wqb, h * 64, QTb[:, h, :], "Wqb", ["QTb"])
        roped(Wkb, 0, KTb[:, blk], "Wkb", [("KTb", b)])
        for h in range(4):
            roped(Wqi, h * 64, QTi[:, h, :], "Wqi", ["QTi"])
        roped(Wki, 0, KTi[:, blk], "Wki", [("KTi", b)])
        for q in range(4):
            t = b * 4 + q
            pb = q % 2
            for kc in range(8):
                mm(kb, C.ps[pb][:, 0:64], C.XT[:, kc, t * 128:(t + 1) * 128], Wvb[:, kc, :], kc == 0, kc == 7, r=[("XT", t), "Wvb"], w=[PB(C, pb)])
            cp(kb, "act", Vb[:, t, 0:64], C.ps[pb][:, 0:64], r=[PB(C, pb)], w=[("V", b)])
            for kc in range(8):
                mm(kb, C.ps[2 + pb][:, 0:4], C.XT[:, kc, t * 128:(t + 1) * 128], Wwi[:, kc, :], kc == 0, kc == 7, r=[("XT", t), "Wwi"], w=[PB(C, 2 + pb)])
            cp(kb, "dve", WI[:, q, :], C.ps[2 + pb][:, 0:4], r=[PB(C, 2 + pb)], w=["WI"])
        kb.barrier()
        for r_ in range(4):
            i = 4 * b + r_
            nk = i + 1
            N2 = nk * 128
            qs = slice(r_ * 128, (r_ + 1) * 128)
            if i >= 2:
                for h in range(4):
                    for c in range((N2 + 511) // 512):
                        n0 = c * 512
                        n1 = min(N2, n0 + 512)
                        bank = C.ps[c % 2]
                        mm(kb, bank[:, 0:n1 - n0], QTi[:, h, qs], KTi[:, n0:n1], True, True, r=["QTi"] + [("KTi", bb) for bb in range(b + 1)], w=[PB(C, c % 2)])
                        act(kb, RL[:, 0:n1 - n0], bank[:, 0:n1 - n0], AF.Relu, r=[PB(C, c % 2)], w=["RL"])
                        stt(kb, SC[:, n0:n1], RL[:, 0:n1 - n0], WI[:, r_, h:h + 1], (C.TB if h == 0 else SC)[:, n0:n1], ALU.mult, ALU.add,
                            r=["RL", "WI", "TB", ("SC", c)], w=[("SC", c)])
                sck = [("SC", c) for c in range((N2 + 511) // 512)]
                tt(kb, "pool", SC[:, i * 128:(i + 1) * 128], SC[:, i * 128:(i + 1) * 128], C.dmask, ALU.add, r=sck + ["dmask"], w=sck)
                red(kb, TAU[:, 0:1], SC[:, 0:N2], ALU.max, r=sck, w=["hi"])
                red(kb, TAU[:, 1:2], SC[:, 0:N2 - 128], ALU.min, r=sck, w=["tau"])
                tt(kb, "dve", TAU[:, 2:3], TAU[:, 0:1], TAU[:, 1:2], ALU.subtract, r=["hi", "tau"], w=["rng"])
                ts(kb, "dve", STEPS[:, 0:NIT + 2], C.pow2[:, 0:NIT + 2], TAU[:, 2:3], None, ALU.mult, None, r=["rng", "pow2"], w=["STEPS"])
                tt(kb, "dve", TAU[:, 3:4], TAU[:, 1:2], STEPS[:, 0:1], ALU.add, r=["tau", "STEPS"], w=["cand"])
                for it in range(NIT):
                    ts(kb, "dve", MASK[:, 0:N2], SC[:, 0:N2], TAU[:, 3:4], None, ALU.is_ge, ALU.add, r=sck + ["cand"], w=["MASK", "cnt"], accum=TAU[:, 4:5])
                    stt(kb, TAU[:, 5:6], TAU[:, 4:5], 255.5, STEPS[:, it:it + 1], ALU.is_ge, ALU.mult, r=["cnt", "STEPS"], w=["inc"])
                    stt(kb, TAU[:, 3:4], TAU[:, 5:6], STEPS[:, it + 1:it + 2], TAU[:, 3:4], ALU.subtract, ALU.add, r=["inc", "STEPS", "cand"], w=["cand"])
                tt(kb, "dve", TAU[:, 1:2], TAU[:, 3:4], STEPS[:, NIT:NIT + 1], ALU.subtract, r=["cand", "STEPS"], w=["tau"])
                ts(kb, "dve", MASK[:, 0:N2], SC[:, 0:N2], TAU[:, 1:2], None, ALU.is_ge, None, r=sck + ["tau"], w=["MASK"])
                for k0 in range(0, nk, 8):
                    k1 = min(nk, k0 + 8)
                    for kt in range(k0, k1):
                        kb.op("pe", lambda e, kt=kt, k0=k0: e.transpose(psb7[:, (kt - k0) * 128:(kt - k0 + 1) * 128], MASK[:, kt * 128:(kt + 1) * 128], C.identb),
                              r=["MASK", "identb"], w=[PB(C, 7)])
                    cp(kb, "act", MT[:, k0:k1, :], psb7[:, 0:(k1 - k0) * 128].rearrange("p (a b) -> p a b", b=128), r=[PB(C, 7)], w=["MT"])
            steps = []
            for hg in range(2):
                otb = 4 + hg
                OTb = C.ps[otb]
                for kt in range(nk):
                    sb_ = C.cnt % 4
                    pi = C.cnt % 4
                    C.cnt += 1

                    def st(hg=hg, kt=kt, sb_=sb_):
                        mm(kb, C.ps[sb_], KTb[:, kt * 128:(kt + 1) * 128], QTb[:, hg * 4:(hg + 1) * 4, qs], True, True, r=[("KTb", kt // 4), "QTb"], w=[PB(C, sb_)])

                    def ex(kt=kt, sb_=sb_, pi=pi):
                        PT3 = C.PT[pi].rearrange("p (a b) -> p a b", b=128)
                        act(kb, C.PT[pi], C.ps[sb_], AF.Exp, r=[PB(C, sb_)], w=[("PT", pi)], scale=0.125)
                        if i >= 2:
                            tt(kb, "dve", PT3, PT3, MT[:, kt:kt + 1, :].broadcast_to([128, 4, 128]), ALU.mult, r=[("PT", pi), "MT"], w=[("PT", pi)])
                        elif kt == i:
                            kb.op("pool", lambda e, PT3=PT3: e.memset(PT3[64:128, :, 0:64], 0.0), r=[("PT", pi)], w=[("PT", pi)])

                    def pv(kt=kt, pi=pi, OTb=OTb, otb=otb):
                        mm(kb, OTb[0:65, :], Vb[:, kt, :], C.PT[pi], kt == 0, kt == nk - 1, r=[("V", kt // 4), ("PT", pi)], w=[PB(C, otb)])

                    sp = dict(st=st, ex=ex, pv=pv)
                    if kt == nk - 1:
                        sp["na"] = lambda OTb=OTb, otb=otb: attn_norm_a(C, OTb, PB(C, otb))
                        sp["nb"] = lambda OTb=OTb, otb=otb, hg=hg: attn_norm_b(C, OTb, PB(C, otb), OTs[:, hg * 4:(hg + 1) * 4, qs], shape3=128)
                    steps.append(sp)
            run_pipelined(steps)
        outproj_block(C, b, 8, OTs, Wo, first)
    kb.barrier()


_CFG = dict(NSEQ=4, S=2048, layers=[0, 1, 2, 3], mla=True, dsa=True)


def kernel(**inputs):
    n = 8
    kb = build(_CFG)
    consts = make_consts()
    x = np.ascontiguousarray(inputs["x"], dtype=np.float32)
    pos = np.ascontiguousarray(inputs["positions"], dtype=np.int32)
    shared = {k: np.ascontiguousarray(v) for k, v in inputs.items() if k not in ("x", "positions")}
    in_maps = []
    for c in range(n):
        m = dict(shared)
        m["x"] = np.ascontiguousarray(x[c * 4:(c + 1) * 4])
        m["positions"] = np.ascontiguousarray(pos[c * 4:(c + 1) * 4])
        m["consts"] = consts
        in_maps.append(m)
    res = run_bass_kernel_spmd(kb.nc, in_maps, core_ids=list(range(n)))
    return np.concatenate([r["out"] for r in res.results], axis=0).astype(np.float32)
```

```python
import numpy as np
import concourse.bass as bass
import concourse.mybir as mybir
from concourse.bass_utils import run_bass_kernel_spmd

F32 = mybir.dt.float32
BF16 = mybir.dt.bfloat16
I32 = mybir.dt.int32
AF = mybir.ActivationFunctionType
ALU = mybir.AluOpType
AX = mybir.AxisListType
DTSZ = {F32: 4, BF16: 2, I32: 4}


class Op:
    __slots__ = ("eng", "fn", "r", "w", "dma", "dsem", "dval", "deps", "tick", "marked", "id", "ep")


class KB:
    def __init__(self):
        self.nc = bass.Bass("TRN2", target_bir_lowering=False)
        self.ops = []
        self.sb_off = 0
        self.sb_max = 0
        self.dma_sems = {}

    def sb(self, name, shape, dt, off=None):
        if not hasattr(self, "arena"):
            self.ARENA = 206 * 1024
            self.arena = self.nc.alloc_sbuf_tensor("arena", [128, self.ARENA // 4], F32)
        nbytes = int(np.prod(shape[1:])) * DTSZ[dt]
        nbytes = (nbytes + 3) // 4 * 4
        if off is None:
            off = self.sb_off
            self.sb_off = (off + nbytes + 63) // 64 * 64
        self.sb_max = max(self.sb_max, off + nbytes)
        assert off % 4 == 0 and off + nbytes <= self.ARENA, (name, off, nbytes)
        ap = self.arena[:, off // 4:(off + nbytes) // 4]
        if dt != F32:
            ap = ap.bitcast(dt)
        n = int(np.prod(shape[1:]))
        ap = ap[:, 0:n]
        if len(shape) == 3:
            ap = ap.rearrange("p (a b) -> p a b", b=shape[2])
        elif len(shape) == 4:
            ap = ap.rearrange("p (a b c) -> p a b c", b=shape[2], c=shape[3])
        if shape[0] != 128:
            ap = ap[0:shape[0]]
        return ap

    def op(self, eng, fn, r=(), w=(), dsem=None):
        o = Op()
        o.eng = eng
        o.fn = fn
        o.r = tuple(r)
        o.w = tuple(w)
        o.dma = dsem is not None
        o.dsem = dsem
        o.dval = 0
        o.deps = ()
        o.tick = 0
        o.marked = False
        o.id = len(self.ops)
        self.ops.append(o)
        return o

    def barrier(self):
        return self.op("barrier", None)

    def epoch(self):
        self.op("barrier", None)
        return self.op("epoch", None)

    def dma(self, q, out, in_, r=(), w=(), dsem="d0"):
        return self.op(q, lambda e: e.dma_start(out=out, in_=in_), r, w, dsem=dsem)

    def emit(self):
        nc = self.nc
        ops = self.ops
        lastw = {}
        readers = {}
        lastop = {}
        lastdma = {}
        for o in ops:
            if o.eng == "epoch":
                lastop = {}
                continue
            if o.eng == "barrier":
                o.deps = list(lastop.values()) + list(lastdma.values())
                for p in o.deps:
                    p.marked = True
                lastw = {}
                readers = {}
                continue
            if o.dma:
                lastdma[o.dsem] = o
            else:
                lastop[o.eng] = o
            deps = {}
            for k in o.r:
                p = lastw.get(k)
                if p is not None:
                    deps[p.id] = "RAW"
            for k in o.w:
                p = lastw.get(k)
                if p is not None and p.id not in deps:
                    deps[p.id] = "WAW"
                for p in readers.get(k, {}).values():
                    if isinstance(p, list):
                        for pp in p:
                            deps.setdefault(pp.id, "WAR")
                    else:
                        deps.setdefault(p.id, "WAR")
            for k in o.r:
                d = readers.setdefault(k, {})
                if o.dma:
                    d.setdefault("dma", []).append(o)
                else:
                    d[o.eng] = o
            for k in o.w:
                lastw[k] = o
                readers[k] = {}
            fd = []
            for pid, kind in deps.items():
                p = ops[pid]
                if p is o:
                    continue
                if (not p.dma) and (not o.dma) and p.eng == o.eng:
                    if o.eng == "pe" or kind != "RAW":
                        continue
                if (not p.dma) and o.dma and p.eng == o.eng and kind != "RAW":
                    pass
                p.marked = True
                fd.append(p)
            o.deps = fd
        cnt = {}
        dcnt = {}
        ep = 0
        self.maxticks = {}
        for o in ops:
            o.ep = ep
            if o.eng == "epoch":
                ep += 1
                cnt = {}
                continue
            if o.eng == "barrier":
                continue
            if o.dma:
                dcnt[o.dsem] = dcnt.get(o.dsem, 0) + 16
                o.dval = dcnt[o.dsem]
            elif o.marked:
                cnt[o.eng] = cnt.get(o.eng, 0) + 1
                o.tick = cnt[o.eng]
                self.maxticks[o.eng] = max(self.maxticks.get(o.eng, 0), o.tick)
        self.nepochs = ep + 1
        engs = {"pe": nc.tensor, "act": nc.scalar, "dve": nc.vector, "pool": nc.gpsimd, "sp": nc.sync}
        sems = {}
        import contextlib
        self._stack = contextlib.ExitStack()
        for o in ops:
            if o.marked and not o.dma and (o.ep, o.eng) not in sems:
                sems[(o.ep, o.eng)] = self._stack.enter_context(nc.semaphore("s_%s_%d" % (o.eng, o.ep)))
        dsems = {}
        for k in dcnt:
            dsems[k] = self._stack.enter_context(nc.semaphore("d_" + str(k)))
        seen = {e: {} for e in engs}
        nwait = 0
        dissued = {}
        for o in ops:
            if o.eng == "epoch":
                continue
            if o.eng == "barrier":
                for en, E in engs.items():
                    sn = seen[en]
                    for p in o.deps:
                        if p.dma:
                            key = ("d", p.dsem); val = p.dval
                        else:
                            if p.eng == en:
                                continue
                            key = ("c", p.eng, p.ep); val = p.tick
                        if sn.get(key, 0) >= val:
                            continue
                        sem = dsems[key[1]] if key[0] == "d" else sems[(key[2], key[1])]
                        E.wait_ge(sem, val)
                        sn[key] = val
                        nwait += 1
                continue
            E = engs[o.eng]
            sn = seen[o.eng]
            need = {}
            for p in o.deps:
                if p.dma:
                    key = ("d", p.dsem)
                    val = dissued[p.dsem]
                else:
                    key = ("c", p.eng, p.ep)
                    val = p.tick
                if sn.get(key, 0) >= val:
                    continue
                if need.get(key, 0) < val:
                    need[key] = val
            for key, val in need.items():
                sem = dsems[key[1]] if key[0] == "d" else sems[(key[2], key[1])]
                E.wait_ge(sem, val)
                sn[key] = val
                nwait += 1
            ins = o.fn(E)
            if o.dma:
                dissued[o.dsem] = o.dval
                ins.then_inc(dsems[o.dsem], 16)
            elif o.marked:
                ins.then_inc(sems[(o.ep, o.eng)], 1)
        self.nwait = nwait
        return nc


import math

D = 1024
ALPHA = 8 ** 0.25
THETA = 10000.0
NIT = 16
C_ID = 0
C_TRI = 128
C_DM = 256
C_TB = 384
C_P2 = C_TB + 2048
C_SEL = C_P2 + 32
C_INV = C_SEL + 16 * 65
CW = C_INV + 4
TWO_PI = 2.0 * math.pi


def make_consts():
    c = np.zeros((128, CW), np.float32)
    c[:, C_ID:C_ID + 128] = np.eye(128)
    s = np.arange(128)[:, None]
    t = np.arange(128)[None, :]
    c[:, C_TRI:C_TRI + 128] = (t >= s)
    c[:, C_DM:C_DM + 128] = np.where((s < 64) & (t >= 64), -1e30, 0.0)
    c[:, C_TB:C_TB + 2048] = -1e-6 * np.arange(2048)[None, :]
    c[:, C_P2:C_P2 + 32] = 2.0 ** -(np.arange(32) + 1.0)
    sel = np.zeros((16, 16, 65))
    for h in range(16):
        sel[h, h, 64] = -8.0
    c[0:16, C_SEL:C_SEL + 16 * 65] = sel.reshape(16, -1)
    p = np.arange(128)
    c[:, C_INV] = np.where((p >= 64) & (p < 96), THETA ** (-2.0 * ((p - 64) % 16) / 32.0), 0.0)
    c[:, C_INV + 1] = np.where(p < 64, THETA ** (-2.0 * (p % 32) / 64.0), 0.0)
    return c


WSPEC = [("x", None, F32), ("positions", None, I32),
         ("ev_w_in", [2, 1024, 1636], F32), ("ev_g_q", [2, 384], F32), ("ev_g_kv", [2, 256], F32),
         ("ev_w_uq", [2, 384, 768], F32), ("ev_w_ukv", [2, 256, 1024], F32), ("ev_w_o", [2, 1024, 1024], F32),
         ("od_w_in", [2, 1024, 3088], F32), ("od_b_f", [2, 16], F32), ("od_w_o", [2, 1024, 1024], F32),
         ("moe_w_grp", [4, 1024, 4], F32), ("moe_b_grp", [4, 4], F32), ("moe_w_sub", [4, 1024, 16], F32),
         ("moe_b_sub", [4, 16], F32), ("moe_w_gate", [4, 16, 1024, 512], F32), ("moe_w_up", [4, 16, 1024, 512], F32),
         ("moe_w_down", [4, 16, 512, 1024], F32), ("ln1_g", [4, 1024], F32), ("ln1_b", [4, 1024], F32),
         ("ln2_g", [4, 1024], F32), ("ln2_b", [4, 1024], F32)]


class Ctx:
    pass


def mm(kb, out, lhsT, rhs, start, stop, r, w):
    kb.op("pe", lambda e: e.matmul(out, lhsT=lhsT, rhs=rhs, start=start, stop=stop), r, w)


def act(kb, out, in_, func, r, w, **kw):
    kb.op("act", lambda e: e.activation(out=out, in_=in_, func=func, **kw), r, w)


def tt(kb, eng, out, in0, in1, op, r, w):
    kb.op(eng, lambda e: e.tensor_tensor(out=out, in0=in0, in1=in1, op=op), r, w)


def ts(kb, eng, out, in0, s1, s2, op0, op1, r, w, accum=None):
    if op1 is None:
        kb.op(eng, lambda e: e.tensor_scalar(out=out, in0=in0, scalar1=s1, scalar2=None, op0=op0), r, w)
    elif accum is None:
        kb.op(eng, lambda e: e.tensor_scalar(out=out, in0=in0, scalar1=s1, scalar2=s2, op0=op0, op1=op1), r, w)
    else:
        kb.op(eng, lambda e: e.tensor_scalar(out=out, in0=in0, scalar1=s1, scalar2=s2, op0=op0, op1=op1, accum_out=accum), r, w)


def stt(kb, out, in0, scalar, in1, op0, op1, r, w):
    kb.op("dve", lambda e: e.scalar_tensor_tensor(out=out, in0=in0, scalar=scalar, in1=in1, op0=op0, op1=op1), r, w)


def cp(kb, eng, out, in_, r, w):
    if eng == "act":
        kb.op("act", lambda e: e.activation(out=out, in_=in_, func=AF.Copy), r, w)
    else:
        kb.op(eng, lambda e: e.tensor_copy(out=out, in_=in_), r, w)


def red(kb, out, in_, op, r, w):
    kb.op("dve", lambda e: e.tensor_reduce(out=out, in_=in_, axis=AX.X, op=op), r, w)


def recip(kb, out, in_, r, w):
    kb.op("dve", lambda e: e.reciprocal(out=out, in_=in_), r, w)


def wload(kb, dst, src, key, r=(), extra_w=()):
    kb.dma("pool", dst, src, r=r, w=[key] + list(extra_w), dsem="w_" + str(key))


def PB(c, i):
    return ("ps", i)


def build(cfg):
    NSEQ = cfg["NSEQ"]
    S = cfg["S"]
    LAYERS = cfg["layers"]
    NT = S // 128
    NB = S // 512
    kb = KB()
    nc = kb.nc
    C = Ctx()
    C.kb = kb
    C.S, C.NT, C.NB = S, NT, NB
    W = {}
    for name, shape, dt in WSPEC:
        if name == "x":
            shape = [NSEQ, S, D]
        if name == "positions":
            shape = [NSEQ, S]
        W[name] = nc.dram_tensor(name, shape, dt, kind="ExternalInput").ap()
    W["consts"] = nc.dram_tensor("consts", [128, CW], F32, kind="ExternalInput").ap()
    OUT = nc.dram_tensor("out", [NSEQ, S, D], F32, kind="ExternalOutput").ap()
    C.W = W
    C.ps = [nc.alloc_psum_tensor("bank%d" % i, [128, 512], F32)[:] for i in range(8)]
    C.X = kb.sb("X", [128, NT, D], F32)
    C.XT = kb.sb("XT", [128, 8, S], BF16)
    C.ident = kb.sb("ident", [128, 128], F32)
    C.identb = kb.sb("identb", [128, 128], BF16)
    C.tri = kb.sb("tri", [128, 128], BF16)
    C.dmask = kb.sb("dmask", [128, 128], F32)
    C.TB = kb.sb("TB", [128, S], BF16)
    C.pow2 = kb.sb("pow2", [128, 32], F32)
    C.inv = kb.sb("inv", [128, 4], F32)
    C.onesf = kb.sb("onesf", [128, 64], F32)
    C.onesb = kb.sb("onesb", [128, 128], BF16)
    cs = W["consts"]
    kb.dma("sp", C.ident, cs[:, C_ID:C_ID + 128], w=["ident"], dsem="c")
    kb.dma("pool", C.identb, cs[:, C_ID:C_ID + 128], w=["identb"], dsem="c2")
    kb.dma("pool", C.tri, cs[:, C_TRI:C_TRI + 128], w=["tri"], dsem="c2")
    kb.dma("sp", C.dmask, cs[:, C_DM:C_DM + 128], w=["dmask"], dsem="c")
    kb.dma("pool", C.TB, cs[:, C_TB:C_TB + S], w=["TB"], dsem="c2")
    kb.dma("sp", C.pow2, cs[:, C_P2:C_P2 + 32], w=["pow2"], dsem="c")
    kb.dma("sp", C.inv, cs[:, C_INV:C_INV + 4], w=["inv"], dsem="c")
    kb.op("dve", lambda e: e.memset(C.onesf, 1.0), w=["onesf"])
    kb.op("dve", lambda e: e.memset(C.onesb, 1.0), w=["onesb"])
    C.base = kb.sb_off
    kb.barrier()
    for seq in range(NSEQ):
        for t in range(NT):
            kb.dma("sp", C.X[:, t, :], W["x"][seq, t * 128:(t + 1) * 128, :], w=[("X", t, 0), ("X", t, 1)], dsem="x%d" % (t % 2))
        for t in range(NT):
            transpose_tile(C, t)
        kb.barrier()
        for li, l in enumerate(LAYERS):
            j = l // 2
            if li % 2 == 0:
                kb.epoch()
            if l % 2 == 1:
                fox_phase(C, j)
            else:
                first = True
                if cfg.get("mla", True):
                    mla_phase(C, j, seq)
                    first = False
                if cfg.get("dsa", True):
                    dsa_phase(C, j, seq, first)
            ln_phase(C, l, 0, None)
            moe_phase(C, l)
            last = (li == len(LAYERS) - 1)
            ln_phase(C, l, 1, (OUT, seq) if last else None)
        kb.barrier()
    kb.op("sp", lambda e: e.nop(), r=["OUT"])
    kb.emit()
    return kb


def transpose_tile(C, t):
    kb = C.kb
    for kc in range(8):
        bank = C.ps[6 + kc // 4]
        kb.op("pe", lambda e, bank=bank, kc=kc: e.transpose(bank[:, (kc % 4) * 128:(kc % 4 + 1) * 128], C.X[:, t, kc * 128:(kc + 1) * 128], C.ident),
              r=[("X", t, kc // 4), "ident"], w=[PB(C, 6 + kc // 4)])
    for hf in range(2):
        cp(kb, "act", C.XT[:, hf * 4:hf * 4 + 4, t * 128:(t + 1) * 128], C.ps[6 + hf].rearrange("p (a b) -> p a b", b=128),
           r=[PB(C, 6 + hf)], w=[("XT", t)])


def ln_phase(C, l, which, outinfo):
    kb = C.kb
    W = C.W
    NT = C.NT
    kb.barrier()
    kb.sb_off = C.base
    LNP = kb.sb("LNP", [128, 2, D], F32)
    BN = kb.sb("BN", [128, 2, 12], F32)
    MV = kb.sb("MV", [128, 2, 4], F32)
    g = W["ln1_g" if which == 0 else "ln2_g"]
    b = W["ln1_b" if which == 0 else "ln2_b"]
    kb.dma("sp", LNP[:, 0, :], g[l:l + 1, :].partition_broadcast(128), w=["LNP"], dsem="lnp")
    kb.dma("sp", LNP[:, 1, :], b[l:l + 1, :].partition_broadcast(128), w=["LNP"], dsem="lnp")
    for t in range(NT):
        p = t % 2
        xk = [("X", t, 0), ("X", t, 1)]
        Xt = C.X[:, t, :]
        for hf in range(2):
            kb.op("dve", lambda e, hf=hf, p=p, t=t: e.bn_stats(out=BN[:, p, hf * 6:hf * 6 + 6], in_=C.X[:, t, hf * 512:(hf + 1) * 512]), r=[("X", t, hf)], w=[("BN", p, hf)])
        kb.op("dve", lambda e, p=p: e.bn_aggr(out=MV[:, p, 0:2], in_=BN[:, p, :]), r=[("BN", p, 0), ("BN", p, 1)], w=[("MV", p)])
        act(kb, MV[:, p, 2:3], MV[:, p, 1:2], AF.Sqrt, r=[("MV", p)], w=[("MV2", p)], bias=1e-5, scale=1.0)
        recip(kb, MV[:, p, 3:4], MV[:, p, 2:3], r=[("MV2", p)], w=[("MV3", p)])
        ts(kb, "dve", Xt, Xt, MV[:, p, 0:1], MV[:, p, 3:4], ALU.subtract, ALU.mult, r=xk + [("MV", p), ("MV3", p)], w=xk)
        tt(kb, "pool", Xt, Xt, LNP[:, 0, :], ALU.mult, r=xk + ["LNP"], w=xk)
        tt(kb, "pool", Xt, Xt, LNP[:, 1, :], ALU.add, r=xk + ["LNP"], w=xk)
        if outinfo is not None:
            OUT, seq = outinfo
            kb.dma("sp", OUT[seq, t * 128:(t + 1) * 128, :], Xt, r=xk, w=["OUT"], dsem="out")
        else:
            transpose_tile(C, t)
            if which == 0:
                ts(kb, "pool", Xt, Xt, ALPHA, 1.0, ALU.mult, ALU.mult, r=xk, w=xk)
    kb.barrier()


def moe_phase(C, l):
    kb = C.kb
    W = C.W
    NT, NB = C.NT, C.NB
    kb.sb_off = C.base
    Wg = [kb.sb("Wg%d" % i, [128, 8, 512], BF16) for i in range(2)]
    Wu = [kb.sb("Wu%d" % i, [128, 8, 512], BF16) for i in range(2)]
    Wd = [kb.sb("Wd%d" % i, [128, 4, 1024], BF16) for i in range(2)]
    Wr = kb.sb("Wr", [128, 8, 20], BF16)
    brow = kb.sb("brow", [128, 20], F32)
    L = kb.sb("L", [128, NT, 20], F32)
    G = kb.sb("G", [128, NT, 16], F32)
    T1 = kb.sb("T1", [128, NT, 16], F32)
    T2 = kb.sb("T2", [128, NT, 16], F32)
    T3 = kb.sb("T3", [128, NT, 16], F32)
    SM = kb.sb("SM", [128, 12, NT], F32)
    hT = [kb.sb("hT%d" % i, [128, 4, 512], BF16) for i in range(2)]
    sg = [kb.sb("sg%d" % i, [128, 512], F32) for i in range(2)]

    def load_expert(e):
        i = e % 2
        wload(kb, Wg[i], W["moe_w_gate"][l, e].rearrange("(kc p) f -> p kc f", p=128), ("Wg", i))
        wload(kb, Wu[i], W["moe_w_up"][l, e].rearrange("(kc p) f -> p kc f", p=128), ("Wu", i))
        wload(kb, Wd[i], W["moe_w_down"][l, e].rearrange("(kc p) f -> p kc f", p=128), ("Wd", i))

    load_expert(0)
    wload(kb, Wr[:, :, 0:4], W["moe_w_grp"][l].rearrange("(kc p) f -> p kc f", p=128), "Wr")
    wload(kb, Wr[:, :, 4:20], W["moe_w_sub"][l].rearrange("(kc p) f -> p kc f", p=128), "Wr")
    kb.dma("sp", brow[:, 0:4], W["moe_b_grp"][l:l + 1, :].partition_broadcast(128), w=["brow"], dsem="brow")
    kb.dma("sp", brow[:, 4:20], W["moe_b_sub"][l:l + 1, :].partition_broadcast(128), w=["brow"], dsem="brow")
    for t in range(NT):
        bank = C.ps[6 + t % 2]
        for kc in range(8):
            mm(kb, bank[:, 0:20], C.XT[:, kc, t * 128:(t + 1) * 128], Wr[:, kc, :], kc == 0, kc == 7, r=[("XT", t), "Wr"], w=[PB(C, 6 + t % 2)])
        tt(kb, "dve", L[:, t, :], bank[:, 0:20], brow, ALU.add, r=[PB(C, 6 + t % 2), "brow"], w=["L"])
    lg = L[:, :, 0:4]
    ls = L[:, :, 4:20]
    m = SM[:, 0, :]
    se = SM[:, 1, :]
    ptop = SM[:, 2, :]
    v1 = SM[:, 3, :]
    v2 = SM[:, 4, :]
    dd = SM[:, 5, :]
    w1 = SM[:, 6, :]
    w1p = SM[:, 7, :]
    w2p = SM[:, 8, :]
    oh = T1[:, :, 0:4]
    sh = T1[:, :, 4:8]
    pen = T1[:, :, 8:12]

    def bc(ap, n):
        return ap.unsqueeze(2).broadcast_to([128, NT, n])

    red(kb, m, lg, ALU.max, r=["L"], w=["m"])
    tt(kb, "dve", oh, lg, bc(m, 4), ALU.is_equal, r=["L", "m"], w=["oh"])
    tt(kb, "dve", sh, lg, bc(m, 4), ALU.subtract, r=["L", "m"], w=["sh"])
    act(kb, sh, sh, AF.Exp, r=["sh"], w=["sh"])
    red(kb, se, sh, ALU.add, r=["sh"], w=["se"])
    recip(kb, ptop, se, r=["se"], w=["ptop"])
    ts(kb, "dve", pen, oh, 1.0, 1e30, ALU.subtract, ALU.mult, r=["oh"], w=["pen"])
    tt(kb, "dve", T2.rearrange("p t (a b) -> p t a b", b=4), ls.rearrange("p t (a b) -> p t a b", b=4),
       pen.unsqueeze(3).broadcast_to([128, NT, 4, 4]), ALU.add, r=["L", "pen"], w=["T2"])
    red(kb, v1, T2, ALU.max, r=["T2"], w=["v1"])
    tt(kb, "dve", T3, T2, bc(v1, 16), ALU.is_equal, r=["T2", "v1"], w=["T3"])
    stt(kb, T2, T3, -1e30, T2, ALU.mult, ALU.add, r=["T3", "T2"], w=["T2"])
    red(kb, v2, T2, ALU.max, r=["T2"], w=["v2"])
    tt(kb, "dve", T2, T2, bc(v2, 16), ALU.is_equal, r=["T2", "v2"], w=["T2"])
    tt(kb, "dve", dd, v2, v1, ALU.subtract, r=["v1", "v2"], w=["dd"])
    act(kb, dd, dd, AF.Exp, r=["dd"], w=["dd"])
    ts(kb, "dve", dd, dd, 1.0, None, ALU.add, None, r=["dd"], w=["dd"])
    recip(kb, w1, dd, r=["dd"], w=["w1"])
    tt(kb, "dve", w1p, w1, ptop, ALU.mult, r=["w1", "ptop"], w=["w1p"])
    tt(kb, "dve", w2p, ptop, w1p, ALU.subtract, r=["w1p", "ptop"], w=["w2p"])
    tt(kb, "dve", G, T3, bc(w1p, 16), ALU.mult, r=["T3", "w1p"], w=["G"])
    tt(kb, "dve", T2, T2, bc(w2p, 16), ALU.mult, r=["T2", "w2p"], w=["T2"])
    tt(kb, "dve", G, G, T2, ALU.add, r=["G", "T2"], w=["G"])
    cnt = 0
    for e in range(16):
        i = e % 2
        if e + 1 < 16:
            load_expert(e + 1)
        for b in range(NB):
            hb = cnt % 2
            cnt += 1
            blk = slice(b * 512, (b + 1) * 512)
            xr = [("XT", b * 4 + q) for q in range(4)]
            for fc in range(4):
                pg = fc % 2
                pu = 2 + fc % 2
                for kc in range(8):
                    mm(kb, C.ps[pg], Wg[i][:, kc, fc * 128:(fc + 1) * 128], C.XT[:, kc, blk], kc == 0, kc == 7, r=xr + [("Wg", i)], w=[PB(C, pg)])
                for kc in range(8):
                    mm(kb, C.ps[pu], Wu[i][:, kc, fc * 128:(fc + 1) * 128], C.XT[:, kc, blk], kc == 0, kc == 7, r=xr + [("Wu", i)], w=[PB(C, pu)])
                act(kb, sg[fc % 2], C.ps[pg], AF.Silu, r=[PB(C, pg)], w=[("sg", fc % 2)])
                tt(kb, "dve", hT[hb][:, fc, :], sg[fc % 2], C.ps[pu], ALU.mult, r=[("sg", fc % 2), PB(C, pu)], w=[("hT", hb, fc)])
            for q in range(4):
                t = b * 4 + q
                for hf in range(2):
                    py = 4 + hf
                    for fc in range(4):
                        mm(kb, C.ps[py], hT[hb][:, fc, q * 128:(q + 1) * 128], Wd[i][:, fc, hf * 512:(hf + 1) * 512], fc == 0, fc == 3,
                           r=[("hT", hb, fc), ("Wd", i)], w=[PB(C, py)])
                    Xh = C.X[:, t, hf * 512:(hf + 1) * 512]
                    stt(kb, Xh, C.ps[py], G[:, t, e:e + 1], Xh, ALU.mult, ALU.add, r=[PB(C, py), "G", ("X", t, hf)], w=[("X", t, hf)])


def attn_norm_a(C, OTb, nrows_key):
    kb = C.kb
    recip(kb, C.RR[64:65, :], OTb[64:65, :], r=[nrows_key], w=["RR"])
    cp(kb, "dve", C.RRb[64:65, 0, :], C.RR[64:65, :], r=["RR"], w=["RRh"])
    tt(kb, "dve", C.RRb[64:65, 1, :], C.RR[64:65, :], C.RRb[64:65, 0, :], ALU.subtract, r=["RR", "RRh"], w=["RRl"])


def attn_norm_b(C, OTb, nrows_key, out_ap, shape3=None):
    kb = C.kb
    mm(kb, C.ps[6][0:64, :], C.onesb[64:65, 0:64], C.RRb[64:65, 0, :], True, False, r=["RRh", "onesb"], w=[PB(C, 6)])
    mm(kb, C.ps[6][0:64, :], C.onesb[64:65, 0:64], C.RRb[64:65, 1, :], False, True, r=["RRl", "onesb"], w=[PB(C, 6)])
    cp(kb, "act", C.BCs, C.ps[6][0:64, :], r=[PB(C, 6)], w=["BCs"])
    if shape3 is None:
        tt(kb, "dve", out_ap, OTb[0:64, :], C.BCs, ALU.mult, r=[nrows_key, "BCs"], w=["OTs"])
    else:
        tt(kb, "dve", out_ap, OTb[0:64, :].rearrange("p (a b) -> p a b", b=shape3), C.BCs.rearrange("p (a b) -> p a b", b=shape3),
           ALU.mult, r=[nrows_key, "BCs"], w=["OTs"])


LOOK = 3


def run_pipelined(steps):
    pend = []
    for n in range(min(LOOK, len(steps))):
        steps[n]["st"]()
    for n, sp in enumerate(steps):
        if n + LOOK < len(steps):
            steps[n + LOOK]["st"]()
        sp["ex"]()
        sp["pv"]()
        newp = []
        for (cnt_, f) in pend:
            if cnt_ <= 0:
                f()
            else:
                newp.append((cnt_ - 1, f))
        pend = newp
        if "na" in sp:
            sp["na"]()
            pend.append((2, sp["nb"]))
    for (_, f) in pend:
        f()


def attn_block(C, b, nh, KT, QT, V, Krows, scale, biasf, diag, OTs):
    kb = C.kb
    nkt = 4 * b + 4
    steps = []
    for hh in range(nh):
        otb = 4 + hh % 2
        OTb = C.ps[otb]
        for kt in range(nkt):
            r_ = kt - 4 * b
            q0 = max(r_, 0) * 128
            nq = 512 - q0
            sb_ = C.cnt % 4
            pi = C.cnt % 4
            C.cnt += 1

            def st(hh=hh, kt=kt, q0=q0, nq=nq, sb_=sb_):
                mm(kb, C.ps[sb_][:, 0:nq], KT[0:Krows, hh, kt * 128:(kt + 1) * 128], QT[0:Krows, hh, q0:512], True, True,
                   r=[("KT", kt // 4), "QT"], w=[PB(C, sb_)])

            def ex(hh=hh, kt=kt, r_=r_, nq=nq, sb_=sb_, pi=pi):
                kw = dict(scale=scale)
                rk = [PB(C, sb_)]
                if biasf is not None:
                    kw["bias"] = biasf(kt, hh)
                    rk.append("CB")
                act(kb, C.PT[pi][:, 0:nq], C.ps[sb_][:, 0:nq], AF.Exp, r=rk, w=[("PT", pi)], **kw)
                if r_ >= 0:
                    if diag == "tri":
                        tt(kb, "pool", C.PT[pi][:, 0:128], C.PT[pi][:, 0:128], C.tri, ALU.mult, r=[("PT", pi), "tri"], w=[("PT", pi)])
                    else:
                        kb.op("pool", lambda e, pi=pi: e.memset(C.PT[pi][64:128, 0:64], 0.0), r=[("PT", pi)], w=[("PT", pi)])

            def pv(hh=hh, kt=kt, q0=q0, nq=nq, pi=pi, OTb=OTb, otb=otb):
                mm(kb, OTb[0:V.shape[3], q0:512], V[:, kt, hh, :], C.PT[pi][:, 0:nq], kt == 0, kt == nkt - 1, r=[("V", kt // 4), ("PT", pi)], w=[PB(C, otb)])

            sp = dict(st=st, ex=ex, pv=pv)
            if kt == nkt - 1:
                sp["na"] = lambda OTb=OTb, otb=otb: attn_norm_a(C, OTb, PB(C, otb))
                sp["nb"] = lambda OTb=OTb, otb=otb, hh=hh: attn_norm_b(C, OTb, PB(C, otb), OTs[:, hh, :])
            steps.append(sp)
    run_pipelined(steps)


def outproj_block(C, b, nh, OTs, Wo, first):
    kb = C.kb
    for q in range(4):
        t = b * 4 + q
        for hf in range(2):
            bi = (q * 2 + hf) % 4
            bank = C.ps[bi]
            for hh in range(nh):
                mm(kb, bank, OTs[:, hh, q * 128:(q + 1) * 128], Wo[:, hh, hf * 512:(hf + 1) * 512], hh == 0, hh == nh - 1, r=["OTs", "Wo"], w=[PB(C, bi)])
            Xh = C.X[:, t, hf * 512:(hf + 1) * 512]
            stt(kb, Xh, Xh, ALPHA if first else 1.0, bank, ALU.mult, ALU.add, r=[PB(C, bi), ("X", t, hf)], w=[("X", t, hf)])


def alloc_attn_common(C):
    kb = C.kb
    C.PT = [kb.sb("PT%d" % i, [128, 512], BF16) for i in range(4)]
    C.RRb = kb.sb("RRb", [128, 2, 512], BF16)
    C.RR = kb.sb("RR", [128, 512], F32)
    C.BCs = C.RR[0:64]
    C.cnt = 0


def fox_phase(C, j):
    kb = C.kb
    W = C.W
    S, NT, NB = C.S, C.NT, C.NB
    kb.barrier()
    kb.sb_off = C.base
    alloc_attn_common(C)
    win = W["od_w_in"][j]
    Wq = kb.sb("fWq", [128, 8, 4, 65], BF16)
    Wk = kb.sb("fWk", [128, 8, 4, 64], BF16)
    Wv = kb.sb("fWv", [128, 8, 256], BF16)
    Wf = kb.sb("fWf", [128, 8, 16], BF16)
    Wo = kb.sb("fWo", [64, 4, 1024], BF16)
    bf = kb.sb("fbf", [16, 2], F32)
    NL = kb.sb("fNL", [16, S], F32)
    CP = kb.sb("fCP", [16, S], F32)
    CPb = kb.sb("fCPb", [16, S], BF16)
    ones16 = kb.sb("fones", [16, 512], F32)
    C.SEL = kb.sb("SEL", [16, 16, 65], BF16)
    kb.dma("pool", C.SEL, W["consts"][0:16, C_SEL:C_SEL + 16 * 65].rearrange("p (a b) -> p a b", b=65), w=["SEL"], dsem="c2")
    CB = kb.sb("fCB", [128, NT, 16], F32)
    KT = kb.sb("fKT", [128, 4, S], BF16)
    V = kb.sb("fV", [128, NT, 4, 128], BF16)
    QT = kb.sb("fQT", [128, 4, 512], BF16)
    OTs = kb.sb("fOTs", [64, 4, 512], BF16)
    kb.op("pool", lambda e: e.memset(Wq, 0.0), w=["Wq"])
    kb.op("pool", lambda e: e.memset(KT[64:128], 0.0), w=[("KT", b) for b in range(NB)])
    kb.op("pool", lambda e: e.memset(KT[64:65], 1.0), w=[("KT", b) for b in range(NB)])
    kb.op("pool", lambda e: e.memset(QT[64:128], 0.0), w=["QT"])
    kb.op("pool", lambda e: e.memset(V, 0.0), w=[("V", b) for b in range(NB)])
    kb.op("pool", lambda e: e.memset(V[:, :, :, 64:65], 1.0), w=[("V", b) for b in range(NB)])
    kb.op("pool", lambda e: e.memset(ones16, 1.0), w=["ones16"])
    wload(kb, Wf, win[:, 3072:3088].rearrange("(kc p) f -> p kc f", p=128), "Wf")
    kb.dma("sp", bf[:, 0:1], W["od_b_f"][j:j + 1, :].rearrange("a h -> h a"), w=["bf"], dsem="bf")
    ts(kb, "dve", bf[:, 1:2], bf[:, 0:1], -1.0, None, ALU.mult, None, r=["bf"], w=["nbf"])
    for b in range(NB):
        blk = slice(b * 512, (b + 1) * 512)
        for kc in range(8):
            mm(kb, C.ps[0][0:16, :], Wf[:, kc, :], C.XT[:, kc, blk], kc == 0, kc == 7, r=[("XT", b * 4 + q) for q in range(4)] + ["Wf"], w=[PB(C, 0)])
        act(kb, NL[:, blk], C.ps[0][0:16, :], AF.Exp, r=[PB(C, 0), "nbf"], w=["NL"], scale=-1.0, bias=bf[:, 1:2])
        act(kb, NL[:, blk], NL[:, blk], AF.Ln, r=["NL"], w=["NL"], bias=1.0, scale=1.0)
        init = 0.0 if b == 0 else CP[:, b * 512 - 1:b * 512]
        kb.op("dve", lambda e, blk=blk, init=init: e.tensor_tensor_scan(out=CP[:, blk], data0=ones16, data1=NL[:, blk], initial=init, op0=ALU.mult, op1=ALU.add),
              r=["NL", "ones16", "CP"], w=["CP"])
    cp(kb, "act", CPb, CP, r=["CP"], w=["CPb"])
    for kt in range(NT):
        kb.op("pe", lambda e, kt=kt: e.transpose(C.ps[1][:, 0:16], CP[:, kt * 128:(kt + 1) * 128], C.ident[0:16, 0:16]), r=["CP", "ident"], w=[PB(C, 1)])
        cp(kb, "dve", CB[:, kt, :], C.ps[1][:, 0:16], r=[PB(C, 1)], w=["CB"])
    for g in range(4):
        for hh in range(4):
            h = g * 4 + hh
            wload(kb, Wq[:, :, hh, 0:64], win[:, h * 64:(h + 1) * 64].rearrange("(kc p) f -> p kc f", p=128), "Wq")
            wload(kb, Wk[:, :, hh, :], win[:, 1024 + h * 64:1024 + (h + 1) * 64].rearrange("(kc p) f -> p kc f", p=128), "Wk")
        wload(kb, Wv, win[:, 2048 + g * 256:2048 + (g + 1) * 256].rearrange("(kc p) f -> p kc f", p=128), "Wv")
        wload(kb, Wo, W["od_w_o"][j][g * 256:(g + 1) * 256, :].rearrange("(h d) n -> d h n", d=64), "Wo")
        for b in range(NB):
            blk = slice(b * 512, (b + 1) * 512)
            xr = [("XT", b * 4 + q) for q in range(4)]
            for hh in range(4):
                h = g * 4 + hh
                pb = hh % 2
                for kc in range(8):
                    mm(kb, C.ps[pb][0:65, :], Wq[:, kc, hh, :], C.XT[:, kc, blk], kc == 0, False, r=xr + ["Wq"], w=[PB(C, pb)])
                mm(kb, C.ps[pb][0:65, :], C.SEL[0:16, h, :], CPb[:, blk], False, True, r=["SEL", "CPb"], w=[PB(C, pb)])
                cp(kb, "act", QT[0:65, hh, :], C.ps[pb][0:65, :], r=[PB(C, pb)], w=["QT"])
            for hh in range(4):
                pb = hh % 2
                for kc in range(8):
                    mm(kb, C.ps[pb][0:64, :], Wk[:, kc, hh, :], C.XT[:, kc, blk], kc == 0, kc == 7, r=xr + ["Wk"], w=[PB(C, pb)])
                cp(kb, "dve", KT[0:64, hh, blk], C.ps[pb][0:64, :], r=[PB(C, pb)], w=[("KT", b)])
            for q in range(4):
                t = b * 4 + q
                pb = q % 2
                for kc in range(8):
                    mm(kb, C.ps[pb][:, 0:256], C.XT[:, kc, t * 128:(t + 1) * 128], Wv[:, kc, :], kc == 0, kc == 7, r=[("XT", t), "Wv"], w=[PB(C, pb)])
                cp(kb, "act", V[:, t, :, 0:64], C.ps[pb][:, 0:256].rearrange("p (a b) -> p a b", b=64), r=[PB(C, pb)], w=[("V", b)])
            attn_block(C, b, 4, KT, QT, V, 128, 0.125, lambda kt, hh, g=g: CB[:, kt, g * 4 + hh:g * 4 + hh + 1], "tri", OTs)
            outproj_block(C, b, 4, OTs, Wo, g == 0)
    kb.barrier()


def rope_tables(C, seq, b, tabs):
    kb = C.kb
    W = C.W
    kA, kF = C.kANG, C.kKF
    kb.dma("sp", C.POSI, W["positions"][seq:seq + 1, b * 512:(b + 1) * 512].partition_broadcast(128), w=["POSI"], dsem="pos")
    cp(kb, "dve", C.POSF, C.POSI, r=["POSI"], w=["POSF"])
    for (ic, nr, COS, SIN) in tabs:
        for which, dst in ((0, SIN), (1, COS)):
            ANG = C.ANG[0:nr]
            if which == 0:
                ts(kb, "dve", ANG, C.POSF[0:nr], C.inv[0:nr, ic:ic + 1], None, ALU.mult, None, r=["POSF", "inv", "ROPE"], w=[kA])
            else:
                ts(kb, "dve", ANG, C.POSF[0:nr], C.inv[0:nr, ic:ic + 1], math.pi / 2, ALU.mult, ALU.add, r=["POSF", "inv", "ROPE"], w=[kA])
            ts(kb, "dve", C.KI[0:nr], ANG, 1.0 / TWO_PI, None, ALU.mult, None, r=[kA], w=["POSI"])
            cp(kb, "dve", C.KF[0:nr], C.KI[0:nr], r=["POSI"], w=[kF])
            stt(kb, ANG, C.KF[0:nr], -TWO_PI, ANG, ALU.mult, ALU.add, r=[kF, kA], w=[kA])
            ts(kb, "dve", ANG, ANG, -3.14159, 3.14159, ALU.max, ALU.min, r=[kA], w=[kA])
            act(kb, dst, ANG, AF.Sin, r=[kA], w=["ROPE"])


def alloc_rope(C, TA, TBf):
    kb = C.kb
    C.POSI = kb.sb("POSI", [128, 512], I32)
    C.POSF = kb.sb("POSF", [128, 512], F32)
    C.KI = C.POSI
    C.KF = TA
    C.ANG = TBf
    C.kKF = "TA"
    C.kANG = "TBf"


def load_rot(kb, dst_plain, dst_rot, src_cols_fn, d, key):
    h = d // 2
    wload(kb, dst_plain, src_cols_fn(0, d), key)
    wload(kb, dst_rot[:, :, 0:h], src_cols_fn(h, d), key)
    wload(kb, dst_rot[:, :, h:d], src_cols_fn(0, h), key)
    ts(kb, "pool", dst_rot[:, :, 0:h], dst_rot[:, :, 0:h], -1.0, 1.0, ALU.mult, ALU.mult, r=[key], w=[key])


def mla_phase(C, j, seq):
    kb = C.kb
    W = C.W
    S, NT, NB = C.S, C.NT, C.NB
    kb.barrier()
    kb.sb_off = C.base
    alloc_attn_common(C)
    win = W["ev_w_in"][j]
    Wcq = kb.sb("mWcq", [128, 8, 384], BF16)
    Wckv = kb.sb("mWckv", [128, 8, 256], BF16)
    Wkr = kb.sb("mWkr", [128, 8, 2, 96], BF16)
    Wuq = kb.sb("mWuq", [128, 3, 8, 96], BF16)
    Wuqr = kb.sb("mWuqr", [128, 3, 8, 96], BF16)
    Wukk = kb.sb("mWukk", [128, 2, 8, 64], BF16)
    Wukv = kb.sb("mWukv", [128, 2, 8, 64], BF16)
    Wo = kb.sb("mWo", [64, 4, 1024], BF16)
    gq = kb.sb("mgq", [128, 3], F32)
    gkv = kb.sb("mgkv", [128, 2], F32)
    CQ = kb.sb("mCQ", [128, 3, 512], F32)
    SQ = [kb.sb("mSQ%d" % i, [128, 512], BF16) for i in range(2)]
    RBC = kb.sb("mRBC", [128, 512], F32)
    CQN = kb.sb("mCQN", [128, 3, 512], BF16)
    CKVN = kb.sb("mCKVN", [128, 2, 512], BF16)
    COS = kb.sb("mCOS", [96, 512], F32)
    SIN = kb.sb("mSIN", [96, 512], F32)
    TAx = kb.sb("mTA", [128, 512], F32)
    TBx = kb.sb("mTB", [128, 512], F32)
    alloc_rope(C, TAx, TBx)
    TA = TAx[0:96]
    TBf = TBx[0:96]
    KT = kb.sb("mKT", [96, 4, S], BF16)
    V = kb.sb("mV", [128, NT, 4, 65], BF16)
    QT = kb.sb("mQT", [96, 4, 512], BF16)
    OTs = kb.sb("mOTs", [64, 4, 512], BF16)
    kb.op("pool", lambda e: e.memset(Wkr, 0.0), w=["Wkr"])
    kb.op("pool", lambda e: e.memset(Wuqr, 0.0), w=["Wuqr"])
    kb.op("pool", lambda e: e.memset(V, 1.0), w=[("V", b) for b in range(NB)])

    def cols(c0, c1):
        return win[:, c0:c1].rearrange("(kc p) f -> p kc f", p=128)

    wload(kb, Wcq, cols(0, 384), "Wcq")
    wload(kb, Wckv, cols(384, 640), "Wckv")
    load_rot(kb, Wkr[:, :, 0, 64:96], Wkr[:, :, 1, 64:96], lambda a, b_: cols(640 + a, 640 + b_), 32, "Wkr")
    uq = W["ev_w_uq"][j]
    for h in range(8):
        wload(kb, Wuq[:, :, h, :], uq[:, h * 96:(h + 1) * 96].rearrange("(kc p) f -> p kc f", p=128), "Wuq")
        wload(kb, Wuqr[:, :, h, 64:80], uq[:, h * 96 + 80:h * 96 + 96].rearrange("(kc p) f -> p kc f", p=128), "Wuqr")
        wload(kb, Wuqr[:, :, h, 80:96], uq[:, h * 96 + 64:h * 96 + 80].rearrange("(kc p) f -> p kc f", p=128), "Wuqr")
    ts(kb, "pool", Wuqr[:, :, :, 64:80], Wuqr[:, :, :, 64:80], -1.0, 1.0, ALU.mult, ALU.mult, r=["Wuqr"], w=["Wuqr"])
    ukv = W["ev_w_ukv"][j]
    for h in range(8):
        wload(kb, Wukk[:, :, h, :], ukv[:, h * 128:h * 128 + 64].rearrange("(kc p) f -> p kc f", p=128), "Wukk")
        wload(kb, Wukv[:, :, h, :], ukv[:, h * 128 + 64:h * 128 + 128].rearrange("(kc p) f -> p kc f", p=128), "Wukv")
    for kc in range(3):
        kb.dma("sp", gq[:, kc:kc + 1], W["ev_g_q"][j:j + 1, kc * 128:(kc + 1) * 128].rearrange("a p -> p a"), w=["gq"], dsem="gq")
    for kc in range(2):
        kb.dma("sp", gkv[:, kc:kc + 1], W["ev_g_kv"][j:j + 1, kc * 128:(kc + 1) * 128].rearrange("a p -> p a"), w=["gkv"], dsem="gq")
    scale = 96.0 ** -0.5
    for g in range(2):
        wload(kb, Wo, W["ev_w_o"][j][g * 256:(g + 1) * 256, :].rearrange("(h d) n -> d h n", d=64), "Wo")
        for b in range(NB):
            blk = slice(b * 512, (b + 1) * 512)
            xr = [("XT", b * 4 + q) for q in range(4)]
            kb.barrier()
            rope_tables(C, seq, b, [(0, 96, COS, SIN)])
            for (Wc, nch, gcol, OUTN, nfeat, eps) in ((Wcq, 3, gq, CQN, 384.0, 1e-6), (Wckv, 2, gkv, CKVN, 256.0, 1e-6)):
                for c in range(nch):
                    pb = c % 2
                    for kc in range(8):
                        mm(kb, C.ps[pb], Wc[:, kc, c * 128:(c + 1) * 128], C.XT[:, kc, blk], kc == 0, kc == 7, r=xr + ["Wcq", "Wckv"], w=[PB(C, pb)])
                    cp(kb, "act", CQ[:, c, :], C.ps[pb], r=[PB(C, pb)], w=[("CQ", c)])
                    act(kb, SQ[c % 2], C.ps[pb], AF.Square, r=[PB(C, pb)], w=[("SQ", c % 2)])
                    mm(kb, C.ps[6], C.onesb, SQ[c % 2], c == 0, c == nch - 1, r=[("SQ", c % 2), "onesb"], w=[PB(C, 6)])
                act(kb, RBC, C.ps[6], AF.Sqrt, r=[PB(C, 6)], w=["RBC"], scale=1.0 / nfeat, bias=eps)
                recip(kb, RBC, RBC, r=["RBC"], w=["RBC"])
                for c in range(nch):
                    stt(kb, OUTN[:, c, :], CQ[:, c, :], gcol[:, c:c + 1], RBC, ALU.mult, ALU.mult, r=[("CQ", c), "RBC", "gq", "gkv"], w=["OUTN"])
            for kc in range(8):
                mm(kb, C.ps[0][0:96, :], Wkr[:, kc, 0, :], C.XT[:, kc, blk], kc == 0, kc == 7, r=xr + ["Wkr"], w=[PB(C, 0)])
            for kc in range(8):
                mm(kb, C.ps[1][0:96, :], Wkr[:, kc, 1, :], C.XT[:, kc, blk], kc == 0, kc == 7, r=xr + ["Wkr"], w=[PB(C, 1)])
            tt(kb, "dve", TA[64:96], C.ps[0][64:96, :], COS[64:96], ALU.mult, r=[PB(C, 0), "ROPE"], w=["TA"])
            tt(kb, "dve", TBf[64:96], C.ps[1][64:96, :], SIN[64:96], ALU.mult, r=[PB(C, 1), "ROPE"], w=["TBf"])
            for hh in range(4):
                tt(kb, "pool", KT[64:96, hh, blk], TA[64:96], TBf[64:96], ALU.add, r=["TA", "TBf"], w=[("KT", b)])
            for hh in range(4):
                h = g * 4 + hh
                pb = hh % 2
                for kc in range(2):
                    mm(kb, C.ps[pb][0:64, :], Wukk[:, kc, h, :], CKVN[:, kc, :], kc == 0, kc == 1, r=["OUTN", "Wukk"], w=[PB(C, pb)])
                cp(kb, "act", KT[0:64, hh, blk], C.ps[pb][0:64, :], r=[PB(C, pb)], w=[("KT", b)])
            for q in range(4):
                t = b * 4 + q
                pb = q % 2
                for kc in range(2):
                    mm(kb, C.ps[pb][:, 0:256], CKVN[:, kc, q * 128:(q + 1) * 128], Wukv[:, kc, g * 4:(g + 1) * 4, :], kc == 0, kc == 1, r=["OUTN", "Wukv"], w=[PB(C, pb)])
                cp(kb, "act", V[:, t, :, 0:64], C.ps[pb][:, 0:256].rearrange("p (a b) -> p a b", b=64), r=[PB(C, pb)], w=[("V", b)])
            for hh in range(4):
                h = g * 4 + hh
                for kc in range(3):
                    mm(kb, C.ps[0][0:96, :], Wuq[:, kc, h, :], CQN[:, kc, :], kc == 0, kc == 2, r=["OUTN", "Wuq"], w=[PB(C, 0)])
                for kc in range(3):
                    mm(kb, C.ps[1][0:96, :], Wuqr[:, kc, h, :], CQN[:, kc, :], kc == 0, kc == 2, r=["OUTN", "Wuqr"], w=[PB(C, 1)])
                tt(kb, "dve", TA, C.ps[0][0:96, :], COS, ALU.mult, r=[PB(C, 0), "ROPE"], w=["TA"])
                tt(kb, "dve", TBf, C.ps[1][0:96, :], SIN, ALU.mult, r=[PB(C, 1), "ROPE"], w=["TBf"])
                tt(kb, "pool", QT[:, hh, :], TA, TBf, ALU.add, r=["TA", "TBf"], w=["QT"])
            attn_block(C, b, 4, KT, QT, V, 96, scale, None, "chunk", OTs)
            outproj_block(C, b, 4, OTs, Wo, g == 0)
    kb.barrier()


def dsa_phase(C, j, seq, first):
    kb = C.kb
    W = C.W
    S, NT, NB = C.S, C.NT, C.NB
    kb.barrier()
    kb.sb_off = C.base
    alloc_attn_common(C)
    win = W["ev_w_in"][j]
    Wqb = kb.sb("dWqb", [128, 8, 2, 512], BF16)
    Wkb = kb.sb("dWkb", [128, 8, 2, 64], BF16)
    Wvb = kb.sb("dWvb", [128, 8, 64], BF16)
    Wqi = kb.sb("dWqi", [128, 8, 2, 256], BF16)
    Wki = kb.sb("dWki", [128, 8, 2, 64], BF16)
    Wwi = kb.sb("dWwi", [128, 8, 4], BF16)
    Wo = kb.sb("dWo", [64, 8, 1024], BF16)
    KTb = kb.sb("dKTb", [64, S], BF16)
    Vb = kb.sb("dVb", [128, NT, 65], BF16)
    KTi = kb.sb("dKTi", [64, S], BF16)
    QTb = kb.sb("dQTb", [64, 8, 512], BF16)
    QTi = kb.sb("dQTi", [64, 4, 512], BF16)
    WI = kb.sb("dWI", [128, 4, 4], F32)
    OTs = kb.sb("dOTs", [64, 8, 512], BF16)
    MT = kb.sb("dMT", [128, NT, 128], BF16)
    TAU = kb.sb("dTAU", [128, 8], F32)
    STEPS = kb.sb("dSTEPS", [128, 32], F32)
    offA = kb.sb_off
    COS = kb.sb("dCOS", [64, 512], F32)
    SIN = kb.sb("dSIN", [64, 512], F32)
    TAx = kb.sb("dTA", [128, 512], F32)
    TBx = kb.sb("dTB", [128, 512], F32)
    alloc_rope(C, TAx, TBx)
    TA = TAx[0:64]
    TBf = TBx[0:64]
    endA = kb.sb_off
    kb.sb_off = offA
    SC = kb.sb("dSC", [128, S], F32)
    RL = kb.sb("dRL", [128, 512], F32)
    MASK = kb.sb("dMASK", [128, S], BF16)
    kb.sb_off = max(endA, kb.sb_off)
    psb7 = C.ps[7].bitcast(BF16)

    def cols(c0, c1):
        return win[:, c0:c1].rearrange("(kc p) f -> p kc f", p=128)

    kb.op("pool", lambda e: e.memset(Vb, 1.0), w=[("V", b) for b in range(NB)])
    for h in range(8):
        load_rot(kb, Wqb[:, :, 0, h * 64:(h + 1) * 64], Wqb[:, :, 1, h * 64:(h + 1) * 64], lambda a, b_, h=h: cols(672 + h * 64 + a, 672 + h * 64 + b_), 64, "Wqb")
    load_rot(kb, Wkb[:, :, 0, :], Wkb[:, :, 1, :], lambda a, b_: cols(1184 + a, 1184 + b_), 64, "Wkb")
    wload(kb, Wvb, cols(1248, 1312), "Wvb")
    for h in range(4):
        load_rot(kb, Wqi[:, :, 0, h * 64:(h + 1) * 64], Wqi[:, :, 1, h * 64:(h + 1) * 64], lambda a, b_, h=h: cols(1312 + h * 64 + a, 1312 + h * 64 + b_), 64, "Wqi")
    load_rot(kb, Wki[:, :, 0, :], Wki[:, :, 1, :], lambda a, b_: cols(1568 + a, 1568 + b_), 64, "Wki")
    wload(kb, Wwi, cols(1632, 1636), "Wwi")
    wload(kb, Wo, W["ev_w_o"][j][512:1024, :].rearrange("(h d) n -> d h n", d=64), "Wo")

    def roped(Wt, c0, dst, key_w, wkeys):
        for kc in range(8):
            mm(kb, C.ps[0][0:64, :], Wt[:, kc, 0, c0:c0 + 64], C.XT[:, kc, roped.blk], kc == 0, kc == 7, r=roped.xr + [key_w], w=[PB(C, 0)])
        for kc in range(8):
            mm(kb, C.ps[1][0:64, :], Wt[:, kc, 1, c0:c0 + 64], C.XT[:, kc, roped.blk], kc == 0, kc == 7, r=roped.xr + [key_w], w=[PB(C, 1)])
        tt(kb, "dve", TA, C.ps[0][0:64, :], COS, ALU.mult, r=[PB(C, 0), "ROPE"], w=["TA"])
        tt(kb, "dve", TBf, C.ps[1][0:64, :], SIN, ALU.mult, r=[PB(C, 1), "ROPE"], w=["TBf"])
        tt(kb, "pool", dst, TA, TBf, ALU.add, r=["TA", "TBf"], w=wkeys)

    for b in range(NB):
        blk = slice(b * 512, (b + 1) * 512)
        roped.blk = blk
        roped.xr = [("XT", b * 4 + q) for q in range(4)]
        kb.barrier()
        rope_tables(C, seq, b, [(1, 64, COS, SIN)])
        for h in range(8):
            roped(Wqb, h * 64, QTb[:, h, :], "Wqb", ["QTb"])
        roped(Wkb, 0, KTb[:, blk], "Wkb", [("KTb", b)])
        for h in range(4):
            roped(Wqi, h * 64, QTi[:, h, :], "Wqi", ["QTi"])
        roped(Wki, 0, KTi[:, blk], "Wki", [("KTi", b)])
        for q in range(4):
            t = b * 4 + q
            pb = q % 2
            for kc in range(8):
                mm(kb, C.ps[pb][:, 0:64], C.XT[:, kc, t * 128:(t + 1) * 128], Wvb[:, kc, :], kc == 0, kc == 7, r=[("XT", t), "Wvb"], w=[PB(C, pb)])
            cp(kb, "act", Vb[:, t, 0:64], C.ps[pb][:, 0:64], r=[PB(C, pb)], w=[("V", b)])
            for kc in range(8):
                mm(kb, C.ps[2 + pb][:, 0:4], C.XT[:, kc, t * 128:(t + 1) * 128], Wwi[:, kc, :], kc == 0, kc == 7, r=[("XT", t), "Wwi"], w=[PB(C, 2 + pb)])
            cp(kb, "dve", WI[:, q, :], C.ps[2 + pb][:, 0:4], r=[PB(C, 2 + pb)], w=["WI"])
        kb.barrier()
        for r_ in range(4):
            i = 4 * b + r_
            nk = i + 1
            N2 = nk * 128
            qs = slice(r_ * 128, (r_ + 1) * 128)
            if i >= 2:
                for h in range(4):
                    for c in range((N2 + 511) // 512):
                        n0 = c * 512
                        n1 = min(N2, n0 + 512)
                        bank = C.ps[c % 2]
                        mm(kb, bank[:, 0:n1 - n0], QTi[:, h, qs], KTi[:, n0:n1], True, True, r=["QTi"] + [("KTi", bb) for bb in range(b + 1)], w=[PB(C, c % 2)])
                        act(kb, RL[:, 0:n1 - n0], bank[:, 0:n1 - n0], AF.Relu, r=[PB(C, c % 2)], w=["RL"])
                        stt(kb, SC[:, n0:n1], RL[:, 0:n1 - n0], WI[:, r_, h:h + 1], (C.TB if h == 0 else SC)[:, n0:n1], ALU.mult, ALU.add,
                            r=["RL", "WI", "TB", ("SC", c)], w=[("SC", c)])
                sck = [("SC", c) for c in range((N2 + 511) // 512)]
                tt(kb, "pool", SC[:, i * 128:(i + 1) * 128], SC[:, i * 128:(i + 1) * 128], C.dmask, ALU.add, r=sck + ["dmask"], w=sck)
                red(kb, TAU[:, 0:1], SC[:, 0:N2], ALU.max, r=sck, w=["hi"])
                red(kb, TAU[:, 1:2], SC[:, 0:N2 - 128], ALU.min, r=sck, w=["tau"])
                tt(kb, "dve", TAU[:, 2:3], TAU[:, 0:1], TAU[:, 1:2], ALU.subtract, r=["hi", "tau"], w=["rng"])
                ts(kb, "dve", STEPS[:, 0:NIT + 2], C.pow2[:, 0:NIT + 2], TAU[:, 2:3], None, ALU.mult, None, r=["rng", "pow2"], w=["STEPS"])
                tt(kb, "dve", TAU[:, 3:4], TAU[:, 1:2], STEPS[:, 0:1], ALU.add, r=["tau", "STEPS"], w=["cand"])
                for it in range(NIT):
                    ts(kb, "dve", MASK[:, 0:N2], SC[:, 0:N2], TAU[:, 3:4], None, ALU.is_ge, ALU.add, r=sck + ["cand"], w=["MASK", "cnt"], accum=TAU[:, 4:5])
                    stt(kb, TAU[:, 5:6], TAU[:, 4:5], 255.5, STEPS[:, it:it + 1], ALU.is_ge, ALU.mult, r=["cnt", "STEPS"], w=["inc"])
                    stt(kb, TAU[:, 3:4], TAU[:, 5:6], STEPS[:, it + 1:it + 2], TAU[:, 3:4], ALU.subtract, ALU.add, r=["inc", "STEPS", "cand"], w=["cand"])
                tt(kb, "dve", TAU[:, 1:2], TAU[:, 3:4], STEPS[:, NIT:NIT + 1], ALU.subtract, r=["cand", "STEPS"], w=["tau"])
                ts(kb, "dve", MASK[:, 0:N2], SC[:, 0:N2], TAU[:, 1:2], None, ALU.is_ge, None, r=sck + ["tau"], w=["MASK"])
                for k0 in range(0, nk, 8):
                    k1 = min(nk, k0 + 8)
                    for kt in range(k0, k1):
                        kb.op("pe", lambda e, kt=kt, k0=k0: e.transpose(psb7[:, (kt - k0) * 128:(kt - k0 + 1) * 128], MASK[:, kt * 128:(kt + 1) * 128], C.identb),
                              r=["MASK", "identb"], w=[PB(C, 7)])
                    cp(kb, "act", MT[:, k0:k1, :], psb7[:, 0:(k1 - k0) * 128].rearrange("p (a b) -> p a b", b=128), r=[PB(C, 7)], w=["MT"])
            steps = []
            for hg in range(2):
                otb = 4 + hg
                OTb = C.ps[otb]
                for kt in range(nk):
                    sb_ = C.cnt % 4
                    pi = C.cnt % 4
                    C.cnt += 1

                    def st(hg=hg, kt=kt, sb_=sb_):
                        mm(kb, C.ps[sb_], KTb[:, kt * 128:(kt + 1) * 128], QTb[:, hg * 4:(hg + 1) * 4, qs], True, True, r=[("KTb", kt // 4), "QTb"], w=[PB(C, sb_)])

                    def ex(kt=kt, sb_=sb_, pi=pi):
                        PT3 = C.PT[pi].rearrange("p (a b) -> p a b", b=128)
                        act(kb, C.PT[pi], C.ps[sb_], AF.Exp, r=[PB(C, sb_)], w=[("PT", pi)], scale=0.125)
                        if i >= 2:
                            tt(kb, "dve", PT3, PT3, MT[:, kt:kt + 1, :].broadcast_to([128, 4, 128]), ALU.mult, r=[("PT", pi), "MT"], w=[("PT", pi)])
                        elif kt == i:
                            kb.op("pool", lambda e, PT3=PT3: e.memset(PT3[64:128, :, 0:64], 0.0), r=[("PT", pi)], w=[("PT", pi)])

                    def pv(kt=kt, pi=pi, OTb=OTb, otb=otb):
                        mm(kb, OTb[0:65, :], Vb[:, kt, :], C.PT[pi], kt == 0, kt == nk - 1, r=[("V", kt // 4), ("PT", pi)], w=[PB(C, otb)])

                    sp = dict(st=st, ex=ex, pv=pv)
                    if kt == nk - 1:
                        sp["na"] = lambda OTb=OTb, otb=otb: attn_norm_a(C, OTb, PB(C, otb))
                        sp["nb"] = lambda OTb=OTb, otb=otb, hg=hg: attn_norm_b(C, OTb, PB(C, otb), OTs[:, hg * 4:(hg + 1) * 4, qs], shape3=128)
                    steps.append(sp)
            run_pipelined(steps)
        outproj_block(C, b, 8, OTs, Wo, first)
    kb.barrier()


_CFG = dict(NSEQ=4, S=2048, layers=[0, 1, 2, 3], mla=True, dsa=True)


def kernel(**inputs):
    n = 8
    kb = build(_CFG)
    consts = make_consts()
    x = np.ascontiguousarray(inputs["x"], dtype=np.float32)
    pos = np.ascontiguousarray(inputs["positions"], dtype=np.int32)
    shared = {k: np.ascontiguousarray(v) for k, v in inputs.items() if k not in ("x", "positions")}
    in_maps = []
    for c in range(n):
        m = dict(shared)
        m["x"] = np.ascontiguousarray(x[c * 4:(c + 1) * 4])
        m["positions"] = np.ascontiguousarray(pos[c * 4:(c + 1) * 4])
        m["consts"] = consts
        in_maps.append(m)
    res = run_bass_kernel_spmd(kb.nc, in_maps, core_ids=list(range(n)))
    return np.concatenate([r["out"] for r in res.results], axis=0).astype(np.float32)
```

```python
import numpy as np
import concourse.bass as bass
import concourse.mybir as mybir
from concourse.bass_utils import run_bass_kernel_spmd

F32 = mybir.dt.float32
BF16 = mybir.dt.bfloat16
I32 = mybir.dt.int32
AF = mybir.ActivationFunctionType
ALU = mybir.AluOpType
AX = mybir.AxisListType
DTSZ = {F32: 4, BF16: 2, I32: 4}


class Op:
    __slots__ = ("eng", "fn", "r", "w", "dma", "dsem", "dval", "deps", "tick", "marked", "id", "ep")


class KB:
    def __init__(self):
        self.nc = bass.Bass("TRN2", target_bir_lowering=False)
        self.ops = []
        self.sb_off = 0
        self.sb_max = 0
        self.dma_sems = {}

    def sb(self, name, shape, dt, off=None):
        if not hasattr(self, "arena"):
            self.ARENA = 206 * 1024
            self.arena = self.nc.alloc_sbuf_tensor("arena", [128, self.ARENA // 4], F32)
        nbytes = int(np.prod(shape[1:])) * DTSZ[dt]
        nbytes = (nbytes + 3) // 4 * 4
        if off is None:
            off = self.sb_off
            self.sb_off = (off + nbytes + 63) // 64 * 64
        self.sb_max = max(self.sb_max, off + nbytes)
        assert off % 4 == 0 and off + nbytes <= self.ARENA, (name, off, nbytes)
        ap = self.arena[:, off // 4:(off + nbytes) // 4]
        if dt != F32:
            ap = ap.bitcast(dt)
        n = int(np.prod(shape[1:]))
        ap = ap[:, 0:n]
        if len(shape) == 3:
            ap = ap.rearrange("p (a b) -> p a b", b=shape[2])
        elif len(shape) == 4:
            ap = ap.rearrange("p (a b c) -> p a b c", b=shape[2], c=shape[3])
        if shape[0] != 128:
            ap = ap[0:shape[0]]
        return ap

    def op(self, eng, fn, r=(), w=(), dsem=None):
        o = Op()
        o.eng = eng
        o.fn = fn
        o.r = tuple(r)
        o.w = tuple(w)
        o.dma = dsem is not None
        o.dsem = dsem
        o.dval = 0
        o.deps = ()
        o.tick = 0
        o.marked = False
        o.id = len(self.ops)
        self.ops.append(o)
        return o

    def barrier(self):
        return self.op("barrier", None)

    def epoch(self):
        self.op("barrier", None)
        return self.op("epoch", None)

    def dma(self, q, out, in_, r=(), w=(), dsem="d0"):
        return self.op(q, lambda e: e.dma_start(out=out, in_=in_), r, w, dsem=dsem)

    def emit(self):
        nc = self.nc
        ops = self.ops
        lastw = {}
        readers = {}
        lastop = {}
        lastdma = {}
        for o in ops:
            if o.eng == "epoch":
                lastop = {}
                continue
            if o.eng == "barrier":
                o.deps = list(lastop.values()) + list(lastdma.values())
                for p in o.deps:
                    p.marked = True
                lastw = {}
                readers = {}
                continue
            if o.dma:
                lastdma[o.dsem] = o
            else:
                lastop[o.eng] = o
            deps = {}
            for k in o.r:
                p = lastw.get(k)
                if p is not None:
                    deps[p.id] = "RAW"
            for k in o.w:
                p = lastw.get(k)
                if p is not None and p.id not in deps:
                    deps[p.id] = "WAW"
                for p in readers.get(k, {}).values():
                    if isinstance(p, list):
                        for pp in p:
                            deps.setdefault(pp.id, "WAR")
                    else:
                        deps.setdefault(p.id, "WAR")
            for k in o.r:
                d = readers.setdefault(k, {})
                if o.dma:
                    d.setdefault("dma", []).append(o)
                else:
                    d[o.eng] = o
            for k in o.w:
                lastw[k] = o
                readers[k] = {}
            fd = []
            for pid, kind in deps.items():
                p = ops[pid]
                if p is o:
                    continue
                if (not p.dma) and (not o.dma) and p.eng == o.eng:
                    if o.eng == "pe" or kind != "RAW":
                        continue
                if (not p.dma) and o.dma and p.eng == o.eng and kind != "RAW":
                    pass
                p.marked = True
                fd.append(p)
            o.deps = fd
        cnt = {}
        dcnt = {}
        ep = 0
        self.maxticks = {}
        for o in ops:
            o.ep = ep
            if o.eng == "epoch":
                ep += 1
                cnt = {}
                continue
            if o.eng == "barrier":
                continue
            if o.dma:
                dcnt[o.dsem] = dcnt.get(o.dsem, 0) + 16
                o.dval = dcnt[o.dsem]
            elif o.marked:
                cnt[o.eng] = cnt.get(o.eng, 0) + 1
                o.tick = cnt[o.eng]
                self.maxticks[o.eng] = max(self.maxticks.get(o.eng, 0), o.tick)
        self.nepochs = ep + 1
        engs = {"pe": nc.tensor, "act": nc.scalar, "dve": nc.vector, "pool": nc.gpsimd, "sp": nc.sync}
        sems = {}
        import contextlib
        self._stack = contextlib.ExitStack()
        for o in ops:
            if o.marked and not o.dma and (o.ep, o.eng) not in sems:
                sems[(o.ep, o.eng)] = self._stack.enter_context(nc.semaphore("s_%s_%d" % (o.eng, o.ep)))
        dsems = {}
        for k in dcnt:
            dsems[k] = self._stack.enter_context(nc.semaphore("d_" + str(k)))
        seen = {e: {} for e in engs}
        nwait = 0
        dissued = {}
        for o in ops:
            if o.eng == "epoch":
                continue
            if o.eng == "barrier":
                for en, E in engs.items():
                    sn = seen[en]
                    for p in o.deps:
                        if p.dma:
                            key = ("d", p.dsem); val = p.dval
                        else:
                            if p.eng == en:
                                continue
                            key = ("c", p.eng, p.ep); val = p.tick
                        if sn.get(key, 0) >= val:
                            continue
                        sem = dsems[key[1]] if key[0] == "d" else sems[(key[2], key[1])]
                        E.wait_ge(sem, val)
                        sn[key] = val
                        nwait += 1
                continue
            E = engs[o.eng]
            sn = seen[o.eng]
            need = {}
            for p in o.deps:
                if p.dma:
                    key = ("d", p.dsem)
                    val = dissued[p.dsem]
                else:
                    key = ("c", p.eng, p.ep)
                    val = p.tick
                if sn.get(key, 0) >= val:
                    continue
                if need.get(key, 0) < val:
                    need[key] = val
            for key, val in need.items():
                sem = dsems[key[1]] if key[0] == "d" else sems[(key[2], key[1])]
                E.wait_ge(sem, val)
                sn[key] = val
                nwait += 1
            ins = o.fn(E)
            if o.dma:
                dissued[o.dsem] = o.dval
                ins.then_inc(dsems[o.dsem], 16)
            elif o.marked:
                ins.then_inc(sems[(o.ep, o.eng)], 1)
        self.nwait = nwait
        return nc


import math

D = 1024
ALPHA = 8 ** 0.25
THETA = 10000.0
NIT = 16
C_ID = 0
C_TRI = 128
C_DM = 256
C_TB = 384
C_P2 = C_TB + 2048
C_SEL = C_P2 + 32
C_INV = C_SEL + 16 * 65
CW = C_INV + 4
TWO_PI = 2.0 * math.pi


def make_consts():
    c = np.zeros((128, CW), np.float32)
    c[:, C_ID:C_ID + 128] = np.eye(128)
    s = np.arange(128)[:, None]
    t = np.arange(128)[None, :]
    c[:, C_TRI:C_TRI + 128] = (t >= s)
    c[:, C_DM:C_DM + 128] = np.where((s < 64) & (t >= 64), -1e30, 0.0)
    c[:, C_TB:C_TB + 2048] = -1e-6 * np.arange(2048)[None, :]
    c[:, C_P2:C_P2 + 32] = 2.0 ** -(np.arange(32) + 1.0)
    sel = np.zeros((16, 16, 65))
    for h in range(16):
        sel[h, h, 64] = -8.0
    c[0:16, C_SEL:C_SEL + 16 * 65] = sel.reshape(16, -1)
    p = np.arange(128)
    c[:, C_INV] = np.where((p >= 64) & (p < 96), THETA ** (-2.0 * ((p - 64) % 16) / 32.0), 0.0)
    c[:, C_INV + 1] = np.where(p < 64, THETA ** (-2.0 * (p % 32) / 64.0), 0.0)
    return c


WSPEC = [("x", None, F32), ("positions", None, I32),
         ("ev_w_in", [2, 1024, 1636], F32), ("ev_g_q", [2, 384], F32), ("ev_g_kv", [2, 256], F32),
         ("ev_w_uq", [2, 384, 768], F32), ("ev_w_ukv", [2, 256, 1024], F32), ("ev_w_o", [2, 1024, 1024], F32),
         ("od_w_in", [2, 1024, 3088], F32), ("od_b_f", [2, 16], F32), ("od_w_o", [2, 1024, 1024], F32),
         ("moe_w_grp", [4, 1024, 4], F32), ("moe_b_grp", [4, 4], F32), ("moe_w_sub", [4, 1024, 16], F32),
         ("moe_b_sub", [4, 16], F32), ("moe_w_gate", [4, 16, 1024, 512], F32), ("moe_w_up", [4, 16, 1024, 512], F32),
         ("moe_w_down", [4, 16, 512, 1024], F32), ("ln1_g", [4, 1024], F32), ("ln1_b", [4, 1024], F32),
         ("ln2_g", [4, 1024], F32), ("ln2_b", [4, 1024], F32)]


class Ctx:
    pass


def mm(kb, out, lhsT, rhs, start, stop, r, w):
    kb.op("pe", lambda e: e.matmul(out, lhsT=lhsT, rhs=rhs, start=start, stop=stop), r, w)


def act(kb, out, in_, func, r, w, **kw):
    kb.op("act", lambda e: e.activation(out=out, in_=in_, func=func, **kw), r, w)


def tt(kb, eng, out, in0, in1, op, r, w):
    kb.op(eng, lambda e: e.tensor_tensor(out=out, in0=in0, in1=in1, op=op), r, w)


def ts(kb, eng, out, in0, s1, s2, op0, op1, r, w, accum=None):
    if op1 is None:
        kb.op(eng, lambda e: e.tensor_scalar(out=out, in0=in0, scalar1=s1, scalar2=None, op0=op0), r, w)
    elif accum is None:
        kb.op(eng, lambda e: e.tensor_scalar(out=out, in0=in0, scalar1=s1, scalar2=s2, op0=op0, op1=op1), r, w)
    else:
        kb.op(eng, lambda e: e.tensor_scalar(out=out, in0=in0, scalar1=s1, scalar2=s2, op0=op0, op1=op1, accum_out=accum), r, w)


def stt(kb, out, in0, scalar, in1, op0, op1, r, w):
    kb.op("dve", lambda e: e.scalar_tensor_tensor(out=out, in0=in0, scalar=scalar, in1=in1, op0=op0, op1=op1), r, w)


def cp(kb, eng, out, in_, r, w):
    if eng == "act":
        kb.op("act", lambda e: e.activation(out=out, in_=in_, func=AF.Copy), r, w)
    else:
        kb.op(eng, lambda e: e.tensor_copy(out=out, in_=in_), r, w)


def red(kb, out, in_, op, r, w):
    kb.op("dve", lambda e: e.tensor_reduce(out=out, in_=in_, axis=AX.X, op=op), r, w)


def recip(kb, out, in_, r, w):
    kb.op("dve", lambda e: e.reciprocal(out=out, in_=in_), r, w)


def wload(kb, dst, src, key, r=(), extra_w=()):
    kb.dma("pool", dst, src, r=r, w=[key] + list(extra_w), dsem="w_" + str(key))


def PB(c, i):
    return ("ps", i)


def build(cfg):
    NSEQ = cfg["NSEQ"]
    S = cfg["S"]
    LAYERS = cfg["layers"]
    NT = S // 128
    NB = S // 512
    kb = KB()
    nc = kb.nc
    C = Ctx()
    C.kb = kb
    C.S, C.NT, C.NB = S, NT, NB
    W = {}
    for name, shape, dt in WSPEC:
        if name == "x":
            shape = [NSEQ, S, D]
        if name == "positions":
            shape = [NSEQ, S]
        W[name] = nc.dram_tensor(name, shape, dt, kind="ExternalInput").ap()
    W["consts"] = nc.dram_tensor("consts", [128, CW], F32, kind="ExternalInput").ap()
    OUT = nc.dram_tensor("out", [NSEQ, S, D], F32, kind="ExternalOutput").ap()
    C.W = W
    C.ps = [nc.alloc_psum_tensor("bank%d" % i, [128, 512], F32)[:] for i in range(8)]
    C.X = kb.sb("X", [128, NT, D], F32)
    C.XT = kb.sb("XT", [128, 8, S], BF16)
    C.ident = kb.sb("ident", [128, 128], F32)
    C.identb = kb.sb("identb", [128, 128], BF16)
    C.tri = kb.sb("tri", [128, 128], BF16)
    C.dmask = kb.sb("dmask", [128, 128], F32)
    C.TB = kb.sb("TB", [128, S], BF16)
    C.pow2 = kb.sb("pow2", [128, 32], F32)
    C.inv = kb.sb("inv", [128, 4], F32)
    C.onesf = kb.sb("onesf", [128, 64], F32)
    C.onesb = kb.sb("onesb", [128, 128], BF16)
    cs = W["consts"]
    kb.dma("sp", C.ident, cs[:, C_ID:C_ID + 128], w=["ident"], dsem="c")
    kb.dma("pool", C.identb, cs[:, C_ID:C_ID + 128], w=["identb"], dsem="c2")
    kb.dma("pool", C.tri, cs[:, C_TRI:C_TRI + 128], w=["tri"], dsem="c2")
    kb.dma("sp", C.dmask, cs[:, C_DM:C_DM + 128], w=["dmask"], dsem="c")
    kb.dma("pool", C.TB, cs[:, C_TB:C_TB + S], w=["TB"], dsem="c2")
    kb.dma("sp", C.pow2, cs[:, C_P2:C_P2 + 32], w=["pow2"], dsem="c")
    kb.dma("sp", C.inv, cs[:, C_INV:C_INV + 4], w=["inv"], dsem="c")
    kb.op("dve", lambda e: e.memset(C.onesf, 1.0), w=["onesf"])
    kb.op("dve", lambda e: e.memset(C.onesb, 1.0), w=["onesb"])
    C.base = kb.sb_off
    kb.barrier()
    for seq in range(NSEQ):
        for t in range(NT):
            kb.dma("sp", C.X[:, t, :], W["x"][seq, t * 128:(t + 1) * 128, :], w=[("X", t, 0), ("X", t, 1)], dsem="x%d" % (t % 2))
        for t in range(NT):
            transpose_tile(C, t)
        kb.barrier()
        for li, l in enumerate(LAYERS):
            j = l // 2
            if li % 2 == 0:
                kb.epoch()
            if l % 2 == 1:
                fox_phase(C, j)
            else:
                first = True
                if cfg.get("mla", True):
                    mla_phase(C, j, seq)
                    first = False
                if cfg.get("dsa", True):
                    dsa_phase(C, j, seq, first)
            ln_phase(C, l, 0, None)
            moe_phase(C, l)
            last = (li == len(LAYERS) - 1)
            ln_phase(C, l, 1, (OUT, seq) if last else None)
        kb.barrier()
    kb.op("sp", lambda e: e.nop(), r=["OUT"])
    kb.emit()
    return kb


def transpose_tile(C, t):
    kb = C.kb
    for kc in range(8):
        bank = C.ps[6 + kc // 4]
        kb.op("pe", lambda e, bank=bank, kc=kc: e.transpose(bank[:, (kc % 4) * 128:(kc % 4 + 1) * 128], C.X[:, t, kc * 128:(kc + 1) * 128], C.ident),
              r=[("X", t, kc // 4), "ident"], w=[PB(C, 6 + kc // 4)])
    for hf in range(2):
        cp(kb, "act", C.XT[:, hf * 4:hf * 4 + 4, t * 128:(t + 1) * 128], C.ps[6 + hf].rearrange("p (a b) -> p a b", b=128),
           r=[PB(C, 6 + hf)], w=[("XT", t)])


def ln_phase(C, l, which, outinfo):
    kb = C.kb
    W = C.W
    NT = C.NT
    kb.barrier()
    kb.sb_off = C.base
    LNP = kb.sb("LNP", [128, 2, D], F32)
    BN = kb.sb("BN", [128, 2, 12], F32)
    MV = kb.sb("MV", [128, 2, 4], F32)
    g = W["ln1_g" if which == 0 else "ln2_g"]
    b = W["ln1_b" if which == 0 else "ln2_b"]
    kb.dma("sp", LNP[:, 0, :], g[l:l + 1, :].partition_broadcast(128), w=["LNP"], dsem="lnp")
    kb.dma("sp", LNP[:, 1, :], b[l:l + 1, :].partition_broadcast(128), w=["LNP"], dsem="lnp")
    for t in range(NT):
        p = t % 2
        xk = [("X", t, 0), ("X", t, 1)]
        Xt = C.X[:, t, :]
        for hf in range(2):
            kb.op("dve", lambda e, hf=hf, p=p, t=t: e.bn_stats(out=BN[:, p, hf * 6:hf * 6 + 6], in_=C.X[:, t, hf * 512:(hf + 1) * 512]), r=[("X", t, hf)], w=[("BN", p, hf)])
        kb.op("dve", lambda e, p=p: e.bn_aggr(out=MV[:, p, 0:2], in_=BN[:, p, :]), r=[("BN", p, 0), ("BN", p, 1)], w=[("MV", p)])
        act(kb, MV[:, p, 2:3], MV[:, p, 1:2], AF.Sqrt, r=[("MV", p)], w=[("MV2", p)], bias=1e-5, scale=1.0)
        recip(kb, MV[:, p, 3:4], MV[:, p, 2:3], r=[("MV2", p)], w=[("MV3", p)])
        ts(kb, "dve", Xt, Xt, MV[:, p, 0:1], MV[:, p, 3:4], ALU.subtract, ALU.mult, r=xk + [("MV", p), ("MV3", p)], w=xk)
        tt(kb, "pool", Xt, Xt, LNP[:, 0, :], ALU.mult, r=xk + ["LNP"], w=xk)
        tt(kb, "pool", Xt, Xt, LNP[:, 1, :], ALU.add, r=xk + ["LNP"], w=xk)
        if outinfo is not None:
            OUT, seq = outinfo
            kb.dma("sp", OUT[seq, t * 128:(t + 1) * 128, :], Xt, r=xk, w=["OUT"], dsem="out")
        else:
            transpose_tile(C, t)
            if which == 0:
                ts(kb, "pool", Xt, Xt, ALPHA, 1.0, ALU.mult, ALU.mult, r=xk, w=xk)
    kb.barrier()


def moe_phase(C, l):
    kb = C.kb
    W = C.W
    NT, NB = C.NT, C.NB
    kb.sb_off = C.base
    Wg = [kb.sb("Wg%d" % i, [128, 8, 512], BF16) for i in range(2)]
    Wu = [kb.sb("Wu%d" % i, [128, 8, 512], BF16) for i in range(2)]
    Wd = [kb.sb("Wd%d" % i, [128, 4, 1024], BF16) for i in range(2)]
    Wr = kb.sb("Wr", [128, 8, 20], BF16)
    brow = kb.sb("brow", [128, 20], F32)
    L = kb.sb("L", [128, NT, 20], F32)
    G = kb.sb("G", [128, NT, 16], F32)
    T1 = kb.sb("T1", [128, NT, 16], F32)
    T2 = kb.sb("T2", [128, NT, 16], F32)
    T3 = kb.sb("T3", [128, NT, 16], F32)
    SM = kb.sb("SM", [128, 12, NT], F32)
    hT = [kb.sb("hT%d" % i, [128, 4, 512], BF16) for i in range(2)]
    sg = [kb.sb("sg%d" % i, [128, 512], F32) for i in range(2)]

    def load_expert(e):
        i = e % 2
        wload(kb, Wg[i], W["moe_w_gate"][l, e].rearrange("(kc p) f -> p kc f", p=128), ("Wg", i))
        wload(kb, Wu[i], W["moe_w_up"][l, e].rearrange("(kc p) f -> p kc f", p=128), ("Wu", i))
        wload(kb, Wd[i], W["moe_w_down"][l, e].rearrange("(kc p) f -> p kc f", p=128), ("Wd", i))

    load_expert(0)
    wload(kb, Wr[:, :, 0:4], W["moe_w_grp"][l].rearrange("(kc p) f -> p kc f", p=128), "Wr")
    wload(kb, Wr[:, :, 4:20], W["moe_w_sub"][l].rearrange("(kc p) f -> p kc f", p=128), "Wr")
    kb.dma("sp", brow[:, 0:4], W["moe_b_grp"][l:l + 1, :].partition_broadcast(128), w=["brow"], dsem="brow")
    kb.dma("sp", brow[:, 4:20], W["moe_b_sub"][l:l + 1, :].partition_broadcast(128), w=["brow"], dsem="brow")
    for t in range(NT):
        bank = C.ps[6 + t % 2]
        for kc in range(8):
            mm(kb, bank[:, 0:20], C.XT[:, kc, t * 128:(t + 1) * 128], Wr[:, kc, :], kc == 0, kc == 7, r=[("XT", t), "Wr"], w=[PB(C, 6 + t % 2)])
        tt(kb, "dve", L[:, t, :], bank[:, 0:20], brow, ALU.add, r=[PB(C, 6 + t % 2), "brow"], w=["L"])
    lg = L[:, :, 0:4]
    ls = L[:, :, 4:20]
    m = SM[:, 0, :]
    se = SM[:, 1, :]
    ptop = SM[:, 2, :]
    v1 = SM[:, 3, :]
    v2 = SM[:, 4, :]
    dd = SM[:, 5, :]
    w1 = SM[:, 6, :]
    w1p = SM[:, 7, :]
    w2p = SM[:, 8, :]
    oh = T1[:, :, 0:4]
    sh = T1[:, :, 4:8]
    pen = T1[:, :, 8:12]

    def bc(ap, n):
        return ap.unsqueeze(2).broadcast_to([128, NT, n])

    red(kb, m, lg, ALU.max, r=["L"], w=["m"])
    tt(kb, "dve", oh, lg, bc(m, 4), ALU.is_equal, r=["L", "m"], w=["oh"])
    tt(kb, "dve", sh, lg, bc(m, 4), ALU.subtract, r=["L", "m"], w=["sh"])
    act(kb, sh, sh, AF.Exp, r=["sh"], w=["sh"])
    red(kb, se, sh, ALU.add, r=["sh"], w=["se"])
    recip(kb, ptop, se, r=["se"], w=["ptop"])
    ts(kb, "dve", pen, oh, 1.0, 1e30, ALU.subtract, ALU.mult, r=["oh"], w=["pen"])
    tt(kb, "dve", T2.rearrange("p t (a b) -> p t a b", b=4), ls.rearrange("p t (a b) -> p t a b", b=4),
       pen.unsqueeze(3).broadcast_to([128, NT, 4, 4]), ALU.add, r=["L", "pen"], w=["T2"])
    red(kb, v1, T2, ALU.max, r=["T2"], w=["v1"])
    tt(kb, "dve", T3, T2, bc(v1, 16), ALU.is_equal, r=["T2", "v1"], w=["T3"])
    stt(kb, T2, T3, -1e30, T2, ALU.mult, ALU.add, r=["T3", "T2"], w=["T2"])
    red(kb, v2, T2, ALU.max, r=["T2"], w=["v2"])
    tt(kb, "dve", T2, T2, bc(v2, 16), ALU.is_equal, r=["T2", "v2"], w=["T2"])
    tt(kb, "dve", dd, v2, v1, ALU.subtract, r=["v1", "v2"], w=["dd"])
    act(kb, dd, dd, AF.Exp, r=["dd"], w=["dd"])
    ts(kb, "dve", dd, dd, 1.0, None, ALU.add, None, r=["dd"], w=["dd"])
    recip(kb, w1, dd, r=["dd"], w=["w1"])
    tt(kb, "dve", w1p, w1, ptop, ALU.mult, r=["w1", "ptop"], w=["w1p"])
    tt(kb, "dve", w2p, ptop, w1p, ALU.subtract, r=["w1p", "ptop"], w=["w2p"])
    tt(kb, "dve", G, T3, bc(w1p, 16), ALU.mult, r=["T3", "w1p"], w=["G"])
    tt(kb, "dve", T2, T2, bc(w2p, 16), ALU.mult, r=["T2", "w2p"], w=["T2"])
    tt(kb, "dve", G, G, T2, ALU.add, r=["G", "T2"], w=["G"])
    cnt = 0
    for e in range(16):
        i = e % 2
        if e + 1 < 16:
            load_expert(e + 1)
        for b in range(NB):
            hb = cnt % 2
            cnt += 1
            blk = slice(b * 512, (b + 1) * 512)
            xr = [("XT", b * 4 + q) for q in range(4)]
            for fc in range(4):
                pg = fc % 2
                pu = 2 + fc % 2
                for kc in range(8):
                    mm(kb, C.ps[pg], Wg[i][:, kc, fc * 128:(fc + 1) * 128], C.XT[:, kc, blk], kc == 0, kc == 7, r=xr + [("Wg", i)], w=[PB(C, pg)])
                for kc in range(8):
                    mm(kb, C.ps[pu], Wu[i][:, kc, fc * 128:(fc + 1) * 128], C.XT[:, kc, blk], kc == 0, kc == 7, r=xr + [("Wu", i)], w=[PB(C, pu)])
                act(kb, sg[fc % 2], C.ps[pg], AF.Silu, r=[PB(C, pg)], w=[("sg", fc % 2)])
                tt(kb, "dve", hT[hb][:, fc, :], sg[fc % 2], C.ps[pu], ALU.mult, r=[("sg", fc % 2), PB(C, pu)], w=[("hT", hb, fc)])
            for q in range(4):
                t = b * 4 + q
                for hf in range(2):
                    py = 4 + hf
                    for fc in range(4):
                        mm(kb, C.ps[py], hT[hb][:, fc, q * 128:(q + 1) * 128], Wd[i][:, fc, hf * 512:(hf + 1) * 512], fc == 0, fc == 3,
                           r=[("hT", hb, fc), ("Wd", i)], w=[PB(C, py)])
                    Xh = C.X[:, t, hf * 512:(hf + 1) * 512]
                    stt(kb, Xh, C.ps[py], G[:, t, e:e + 1], Xh, ALU.mult, ALU.add, r=[PB(C, py), "G", ("X", t, hf)], w=[("X", t, hf)])


def attn_norm_a(C, OTb, nrows_key):
    kb = C.kb
    recip(kb, C.RR[64:65, :], OTb[64:65, :], r=[nrows_key], w=["RR"])
    cp(kb, "dve", C.RRb[64:65, 0, :], C.RR[64:65, :], r=["RR"], w=["RRh"])
    tt(kb, "dve", C.RRb[64:65, 1, :], C.RR[64:65, :], C.RRb[64:65, 0, :], ALU.subtract, r=["RR", "RRh"], w=["RRl"])


def attn_norm_b(C, OTb, nrows_key, out_ap, shape3=None):
    kb = C.kb
    mm(kb, C.ps[6][0:64, :], C.onesb[64:65, 0:64], C.RRb[64:65, 0, :], True, False, r=["RRh", "onesb"], w=[PB(C, 6)])
    mm(kb, C.ps[6][0:64, :], C.onesb[64:65, 0:64], C.RRb[64:65, 1, :], False, True, r=["RRl", "onesb"], w=[PB(C, 6)])
    cp(kb, "act", C.BCs, C.ps[6][0:64, :], r=[PB(C, 6)], w=["BCs"])
    if shape3 is None:
        tt(kb, "dve", out_ap, OTb[0:64, :], C.BCs, ALU.mult, r=[nrows_key, "BCs"], w=["OTs"])
    else:
        tt(kb, "dve", out_ap, OTb[0:64, :].rearrange("p (a b) -> p a b", b=shape3), C.BCs.rearrange("p (a b) -> p a b", b=shape3),
           ALU.mult, r=[nrows_key, "BCs"], w=["OTs"])


LOOK = 3


def run_pipelined(steps):
    pend = []
    for n in range(min(LOOK, len(steps))):
        steps[n]["st"]()
    for n, sp in enumerate(steps):
        if n + LOOK < len(steps):
            steps[n + LOOK]["st"]()
        sp["ex"]()
        sp["pv"]()
        newp = []
        for (cnt_, f) in pend:
            if cnt_ <= 0:
                f()
            else:
                newp.append((cnt_ - 1, f))
        pend = newp
        if "na" in sp:
            sp["na"]()
            pend.append((2, sp["nb"]))
    for (_, f) in pend:
        f()


def attn_block(C, b, nh, KT, QT, V, Krows, scale, biasf, diag, OTs):
    kb = C.kb
    nkt = 4 * b + 4
    steps = []
    for hh in range(nh):
        otb = 4 + hh % 2
        OTb = C.ps[otb]
        for kt in range(nkt):
            r_ = kt - 4 * b
            q0 = max(r_, 0) * 128
            nq = 512 - q0
            sb_ = C.cnt % 4
            pi = C.cnt % 4
            C.cnt += 1

            def st(hh=hh, kt=kt, q0=q0, nq=nq, sb_=sb_):
                mm(kb, C.ps[sb_][:, 0:nq], KT[0:Krows, hh, kt * 128:(kt + 1) * 128], QT[0:Krows, hh, q0:512], True, True,
                   r=[("KT", kt // 4), "QT"], w=[PB(C, sb_)])

            def ex(hh=hh, kt=kt, r_=r_, nq=nq, sb_=sb_, pi=pi):
                kw = dict(scale=scale)
                rk = [PB(C, sb_)]
                if biasf is not None:
                    kw["bias"] = biasf(kt, hh)
                    rk.append("CB")
                act(kb, C.PT[pi][:, 0:nq], C.ps[sb_][:, 0:nq], AF.Exp, r=rk, w=[("PT", pi)], **kw)
                if r_ >= 0:
                    if diag == "tri":
                        tt(kb, "pool", C.PT[pi][:, 0:128], C.PT[pi][:, 0:128], C.tri, ALU.mult, r=[("PT", pi), "tri"], w=[("PT", pi)])
                    else:
                        kb.op("pool", lambda e, pi=pi: e.memset(C.PT[pi][64:128, 0:64], 0.0), r=[("PT", pi)], w=[("PT", pi)])

            def pv(hh=hh, kt=kt, q0=q0, nq=nq, pi=pi, OTb=OTb, otb=otb):
                mm(kb, OTb[0:V.shape[3], q0:512], V[:, kt, hh, :], C.PT[pi][:, 0:nq], kt == 0, kt == nkt - 1, r=[("V", kt // 4), ("PT", pi)], w=[PB(C, otb)])

            sp = dict(st=st, ex=ex, pv=pv)
            if kt == nkt - 1:
                sp["na"] = lambda OTb=OTb, otb=otb: attn_norm_a(C, OTb, PB(C, otb))
                sp["nb"] = lambda OTb=OTb, otb=otb, hh=hh: attn_norm_b(C, OTb, PB(C, otb), OTs[:, hh, :])
            steps.append(sp)
    run_pipelined(steps)


def outproj_block(C, b, nh, OTs, Wo, first):
    kb = C.kb
    for q in range(4):
        t = b * 4 + q
        for hf in range(2):
            bi = (q * 2 + hf) % 4
            bank = C.ps[bi]
            for hh in range(nh):
                mm(kb, bank, OTs[:, hh, q * 128:(q + 1) * 128], Wo[:, hh, hf * 512:(hf + 1) * 512], hh == 0, hh == nh - 1, r=["OTs", "Wo"], w=[PB(C, bi)])
            Xh = C.X[:, t, hf * 512:(hf + 1) * 512]
            stt(kb, Xh, Xh, ALPHA if first else 1.0, bank, ALU.mult, ALU.add, r=[PB(C, bi), ("X", t, hf)], w=[("X", t, hf)])


def alloc_attn_common(C):
    kb = C.kb
    C.PT = [kb.sb("PT%d" % i, [128, 512], BF16) for i in range(4)]
    C.RRb = kb.sb("RRb", [128, 2, 512], BF16)
    C.RR = kb.sb("RR", [128, 512], F32)
    C.BCs = C.RR[0:64]
    C.cnt = 0


def fox_phase(C, j):
    kb = C.kb
    W = C.W
    S, NT, NB = C.S, C.NT, C.NB
    kb.barrier()
    kb.sb_off = C.base
    alloc_attn_common(C)
    win = W["od_w_in"][j]
    Wq = kb.sb("fWq", [128, 8, 4, 128], BF16)
    Wk = kb.sb("fWk", [128, 8, 4, 128], BF16)
    Wv = kb.sb("fWv", [128, 8, 256], BF16)
    Wf = kb.sb("fWf", [128, 8, 16], BF16)
    Wo = kb.sb("fWo", [64, 4, 1024], BF16)
    bf = kb.sb("fbf", [16, 2], F32)
    NL = kb.sb("fNL", [16, S], F32)
    CP = kb.sb("fCP", [16, S], F32)
    CPb = kb.sb("fCPb", [16, S], BF16)
    ones16 = kb.sb("fones", [16, 512], F32)
    C.SEL = kb.sb("SEL", [16, 16, 65], BF16)
    kb.dma("pool", C.SEL, W["consts"][0:16, C_SEL:C_SEL + 16 * 65].rearrange("p (a b) -> p a b", b=65), w=["SEL"], dsem="c2")
    CB = kb.sb("fCB", [128, NT, 16], F32)
    KT = kb.sb("fKT", [128, 4, S], BF16)
    V = kb.sb("fV", [128, NT, 4, 128], BF16)
    QT = kb.sb("fQT", [128, 4, 512], BF16)
    OTs = kb.sb("fOTs", [64, 4, 512], BF16)
    kb.op("pool", lambda e: e.memset(Wq, 0.0), w=["Wq"])
    kb.op("pool", lambda e: e.memset(Wk, 0.0), w=["Wk"])
    kb.op("pool", lambda e: e.memset(KT[64:128], 0.0), w=[("KT", b) for b in range(NB)])
    kb.op("pool", lambda e: e.memset(KT[64:65], 1.0), w=[("KT", b) for b in range(NB)])
    kb.op("pool", lambda e: e.memset(QT[64:128], 0.0), w=["QT"])
    kb.op("pool", lambda e: e.memset(V, 0.0), w=[("V", b) for b in range(NB)])
    kb.op("pool", lambda e: e.memset(V[:, :, :, 64:65], 1.0), w=[("V", b) for b in range(NB)])
    kb.op("pool", lambda e: e.memset(ones16, 1.0), w=["ones16"])
    wload(kb, Wf, win[:, 3072:3088].rearrange("(kc p) f -> p kc f", p=128), "Wf")
    kb.dma("sp", bf[:, 0:1], W["od_b_f"][j:j + 1, :].rearrange("a h -> h a"), w=["bf"], dsem="bf")
    ts(kb, "dve", bf[:, 1:2], bf[:, 0:1], -1.0, None, ALU.mult, None, r=["bf"], w=["nbf"])
    for b in range(NB):
        blk = slice(b * 512, (b + 1) * 512)
        for kc in range(8):
            mm(kb, C.ps[0][0:16, :], Wf[:, kc, :], C.XT[:, kc, blk], kc == 0, kc == 7, r=[("XT", b * 4 + q) for q in range(4)] + ["Wf"], w=[PB(C, 0)])
        act(kb, NL[:, blk], C.ps[0][0:16, :], AF.Exp, r=[PB(C, 0), "nbf"], w=["NL"], scale=-1.0, bias=bf[:, 1:2])
        act(kb, NL[:, blk], NL[:, blk], AF.Ln, r=["NL"], w=["NL"], bias=1.0, scale=1.0)
        init = 0.0 if b == 0 else CP[:, b * 512 - 1:b * 512]
        kb.op("dve", lambda e, blk=blk, init=init: e.tensor_tensor_scan(out=CP[:, blk], data0=ones16, data1=NL[:, blk], initial=init, op0=ALU.mult, op1=ALU.add),
              r=["NL", "ones16", "CP"], w=["CP"])
    cp(kb, "act", CPb, CP, r=["CP"], w=["CPb"])
    for kt in range(NT):
        kb.op("pe", lambda e, kt=kt: e.transpose(C.ps[1][:, 0:16], CP[:, kt * 128:(kt + 1) * 128], C.ident[0:16, 0:16]), r=["CP", "ident"], w=[PB(C, 1)])
        cp(kb, "dve", CB[:, kt, :], C.ps[1][:, 0:16], r=[PB(C, 1)], w=["CB"])
    for g in range(4):
        for hh in range(4):
            h = g * 4 + hh
            wload(kb, Wq[:, :, hh, 0:64], win[:, h * 64:(h + 1) * 64].rearrange("(kc p) f -> p kc f", p=128), "Wq")
            wload(kb, Wk[:, :, hh, 0:64], win[:, 1024 + h * 64:1024 + (h + 1) * 64].rearrange("(kc p) f -> p kc f", p=128), "Wk")
        wload(kb, Wv, win[:, 2048 + g * 256:2048 + (g + 1) * 256].rearrange("(kc p) f -> p kc f", p=128), "Wv")
        wload(kb, Wo, W["od_w_o"][j][g * 256:(g + 1) * 256, :].rearrange("(h d) n -> d h n", d=64), "Wo")
        for b in range(NB):
            blk = slice(b * 512, (b + 1) * 512)
            xr = [("XT", b * 4 + q) for q in range(4)]
            for hh in range(4):
                h = g * 4 + hh
                pb = hh % 2
                for kc in range(8):
                    mm(kb, C.ps[pb], Wq[:, kc, hh, :], C.XT[:, kc, blk], kc == 0, False, r=xr + ["Wq"], w=[PB(C, pb)])
                mm(kb, C.ps[pb][0:65, :], C.SEL[0:16, h, :], CPb[:, blk], False, True, r=["SEL", "CPb"], w=[PB(C, pb)])
                cp(kb, "act", QT[0:65, hh, :], C.ps[pb][0:65, :], r=[PB(C, pb)], w=["QT"])
            for hh in range(4):
                pb = hh % 2
                for kc in range(8):
                    mm(kb, C.ps[pb], Wk[:, kc, hh, :], C.XT[:, kc, blk], kc == 0, kc == 7, r=xr + ["Wk"], w=[PB(C, pb)])
                cp(kb, "dve", KT[0:64, hh, blk], C.ps[pb][0:64, :], r=[PB(C, pb)], w=[("KT", b)])
            for q in range(4):
                t = b * 4 + q
                pb = q % 2
                for kc in range(8):
                    mm(kb, C.ps[pb][:, 0:256], C.XT[:, kc, t * 128:(t + 1) * 128], Wv[:, kc, :], kc == 0, kc == 7, r=[("XT", t), "Wv"], w=[PB(C, pb)])
                cp(kb, "act", V[:, t, :, 0:64], C.ps[pb][:, 0:256].rearrange("p (a b) -> p a b", b=64), r=[PB(C, pb)], w=[("V", b)])
            attn_block(C, b, 4, KT, QT, V, 128, 0.125, lambda kt, hh, g=g: CB[:, kt, g * 4 + hh:g * 4 + hh + 1], "tri", OTs)
            outproj_block(C, b, 4, OTs, Wo, g == 0)
    kb.barrier()


def rope_tables(C, seq, b, tabs):
    kb = C.kb
    W = C.W
    kA, kF = C.kANG, C.kKF
    kb.dma("sp", C.POSI, W["positions"][seq:seq + 1, b * 512:(b + 1) * 512].partition_broadcast(128), w=["POSI"], dsem="pos")
    cp(kb, "dve", C.POSF, C.POSI, r=["POSI"], w=["POSF"])
    for (ic, nr, COS, SIN) in tabs:
        for which, dst in ((0, SIN), (1, COS)):
            ANG = C.ANG[0:nr]
            if which == 0:
                ts(kb, "dve", ANG, C.POSF[0:nr], C.inv[0:nr, ic:ic + 1], None, ALU.mult, None, r=["POSF", "inv", "ROPE"], w=[kA])
            else:
                ts(kb, "dve", ANG, C.POSF[0:nr], C.inv[0:nr, ic:ic + 1], math.pi / 2, ALU.mult, ALU.add, r=["POSF", "inv", "ROPE"], w=[kA])
            ts(kb, "dve", C.KI[0:nr], ANG, 1.0 / TWO_PI, None, ALU.mult, None, r=[kA], w=["POSI"])
            cp(kb, "dve", C.KF[0:nr], C.KI[0:nr], r=["POSI"], w=[kF])
            stt(kb, ANG, C.KF[0:nr], -TWO_PI, ANG, ALU.mult, ALU.add, r=[kF, kA], w=[kA])
            ts(kb, "dve", ANG, ANG, -3.14159, 3.14159, ALU.max, ALU.min, r=[kA], w=[kA])
            act(kb, dst, ANG, AF.Sin, r=[kA], w=["ROPE"])


def alloc_rope(C, TA, TBf):
    kb = C.kb
    C.POSI = kb.sb("POSI", [128, 512], I32)
    C.POSF = kb.sb("POSF", [128, 512], F32)
    C.KI = C.POSI
    C.KF = TA
    C.ANG = TBf
    C.kKF = "TA"
    C.kANG = "TBf"


def load_rot(kb, dst_plain, dst_rot, src_cols_fn, d, key):
    h = d // 2
    wload(kb, dst_plain, src_cols_fn(0, d), key)
    wload(kb, dst_rot[:, :, 0:h], src_cols_fn(h, d), key)
    wload(kb, dst_rot[:, :, h:d], src_cols_fn(0, h), key)
    ts(kb, "pool", dst_rot[:, :, 0:h], dst_rot[:, :, 0:h], -1.0, 1.0, ALU.mult, ALU.mult, r=[key], w=[key])


def mla_phase(C, j, seq):
    kb = C.kb
    W = C.W
    S, NT, NB = C.S, C.NT, C.NB
    kb.barrier()
    kb.sb_off = C.base
    alloc_attn_common(C)
    win = W["ev_w_in"][j]
    Wcq = kb.sb("mWcq", [128, 8, 384], BF16)
    Wckv = kb.sb("mWckv", [128, 8, 256], BF16)
    Wkr = kb.sb("mWkr", [128, 8, 2, 96], BF16)
    Wuq = kb.sb("mWuq", [128, 3, 8, 96], BF16)
    Wuqr = kb.sb("mWuqr", [128, 3, 8, 96], BF16)
    Wukk = kb.sb("mWukk", [128, 2, 8, 64], BF16)
    Wukv = kb.sb("mWukv", [128, 2, 8, 64], BF16)
    Wo = kb.sb("mWo", [64, 4, 1024], BF16)
    gq = kb.sb("mgq", [128, 3], F32)
    gkv = kb.sb("mgkv", [128, 2], F32)
    CQ = kb.sb("mCQ", [128, 3, 512], F32)
    SQ = [kb.sb("mSQ%d" % i, [128, 512], BF16) for i in range(2)]
    RBC = kb.sb("mRBC", [128, 512], F32)
    CQN = kb.sb("mCQN", [128, 3, 512], BF16)
    CKVN = kb.sb("mCKVN", [128, 2, 512], BF16)
    COS = kb.sb("mCOS", [96, 512], F32)
    SIN = kb.sb("mSIN", [96, 512], F32)
    TAx = kb.sb("mTA", [128, 512], F32)
    TBx = kb.sb("mTB", [128, 512], F32)
    alloc_rope(C, TAx, TBx)
    TA = TAx[0:96]
    TBf = TBx[0:96]
    KT = kb.sb("mKT", [96, 4, S], BF16)
    V = kb.sb("mV", [128, NT, 4, 65], BF16)
    QT = kb.sb("mQT", [96, 4, 512], BF16)
    OTs = kb.sb("mOTs", [64, 4, 512], BF16)
    kb.op("pool", lambda e: e.memset(Wkr, 0.0), w=["Wkr"])
    kb.op("pool", lambda e: e.memset(Wuqr, 0.0), w=["Wuqr"])
    kb.op("pool", lambda e: e.memset(V, 1.0), w=[("V", b) for b in range(NB)])

    def cols(c0, c1):
        return win[:, c0:c1].rearrange("(kc p) f -> p kc f", p=128)

    wload(kb, Wcq, cols(0, 384), "Wcq")
    wload(kb, Wckv, cols(384, 640), "Wckv")
    load_rot(kb, Wkr[:, :, 0, 64:96], Wkr[:, :, 1, 64:96], lambda a, b_: cols(640 + a, 640 + b_), 32, "Wkr")
    uq = W["ev_w_uq"][j]
    for h in range(8):
        wload(kb, Wuq[:, :, h, :], uq[:, h * 96:(h + 1) * 96].rearrange("(kc p) f -> p kc f", p=128), "Wuq")
        wload(kb, Wuqr[:, :, h, 64:80], uq[:, h * 96 + 80:h * 96 + 96].rearrange("(kc p) f -> p kc f", p=128), "Wuqr")
        wload(kb, Wuqr[:, :, h, 80:96], uq[:, h * 96 + 64:h * 96 + 80].rearrange("(kc p) f -> p kc f", p=128), "Wuqr")
    ts(kb, "pool", Wuqr[:, :, :, 64:80], Wuqr[:, :, :, 64:80], -1.0, 1.0, ALU.mult, ALU.mult, r=["Wuqr"], w=["Wuqr"])
    ukv = W["ev_w_ukv"][j]
    for h in range(8):
        wload(kb, Wukk[:, :, h, :], ukv[:, h * 128:h * 128 + 64].rearrange("(kc p) f -> p kc f", p=128), "Wukk")
        wload(kb, Wukv[:, :, h, :], ukv[:, h * 128 + 64:h * 128 + 128].rearrange("(kc p) f -> p kc f", p=128), "Wukv")
    for kc in range(3):
        kb.dma("sp", gq[:, kc:kc + 1], W["ev_g_q"][j:j + 1, kc * 128:(kc + 1) * 128].rearrange("a p -> p a"), w=["gq"], dsem="gq")
    for kc in range(2):
        kb.dma("sp", gkv[:, kc:kc + 1], W["ev_g_kv"][j:j + 1, kc * 128:(kc + 1) * 128].rearrange("a p -> p a"), w=["gkv"], dsem="gq")
    scale = 96.0 ** -0.5
    for g in range(2):
        wload(kb, Wo, W["ev_w_o"][j][g * 256:(g + 1) * 256, :].rearrange("(h d) n -> d h n", d=64), "Wo")
        for b in range(NB):
            blk = slice(b * 512, (b + 1) * 512)
            xr = [("XT", b * 4 + q) for q in range(4)]
            kb.barrier()
            rope_tables(C, seq, b, [(0, 96, COS, SIN)])
            for (Wc, nch, gcol, OUTN, nfeat, eps) in ((Wcq, 3, gq, CQN, 384.0, 1e-6), (Wckv, 2, gkv, CKVN, 256.0, 1e-6)):
                for c in range(nch):
                    pb = c % 2
                    for kc in range(8):
                        mm(kb, C.ps[pb], Wc[:, kc, c * 128:(c + 1) * 128], C.XT[:, kc, blk], kc == 0, kc == 7, r=xr + ["Wcq", "Wckv"], w=[PB(C, pb)])
                    cp(kb, "act", CQ[:, c, :], C.ps[pb], r=[PB(C, pb)], w=[("CQ", c)])
                    act(kb, SQ[c % 2], C.ps[pb], AF.Square, r=[PB(C, pb)], w=[("SQ", c % 2)])
                    mm(kb, C.ps[6], C.onesb, SQ[c % 2], c == 0, c == nch - 1, r=[("SQ", c % 2), "onesb"], w=[PB(C, 6)])
                act(kb, RBC, C.ps[6], AF.Sqrt, r=[PB(C, 6)], w=["RBC"], scale=1.0 / nfeat, bias=eps)
                recip(kb, RBC, RBC, r=["RBC"], w=["RBC"])
                for c in range(nch):
                    stt(kb, OUTN[:, c, :], CQ[:, c, :], gcol[:, c:c + 1], RBC, ALU.mult, ALU.mult, r=[("CQ", c), "RBC", "gq", "gkv"], w=["OUTN"])
            for kc in range(8):
                mm(kb, C.ps[0][0:96, :], Wkr[:, kc, 0, :], C.XT[:, kc, blk], kc == 0, kc == 7, r=xr + ["Wkr"], w=[PB(C, 0)])
            for kc in range(8):
                mm(kb, C.ps[1][0:96, :], Wkr[:, kc, 1, :], C.XT[:, kc, blk], kc == 0, kc == 7, r=xr + ["Wkr"], w=[PB(C, 1)])
            tt(kb, "dve", TA[64:96], C.ps[0][64:96, :], COS[64:96], ALU.mult, r=[PB(C, 0), "ROPE"], w=["TA"])
            tt(kb, "dve", TBf[64:96], C.ps[1][64:96, :], SIN[64:96], ALU.mult, r=[PB(C, 1), "ROPE"], w=["TBf"])
            for hh in range(4):
                tt(kb, "pool", KT[64:96, hh, blk], TA[64:96], TBf[64:96], ALU.add, r=["TA", "TBf"], w=[("KT", b)])
            for hh in range(4):
                h = g * 4 + hh
                pb = hh % 2
                for kc in range(2):
                    mm(kb, C.ps[pb][0:64, :], Wukk[:, kc, h, :], CKVN[:, kc, :], kc == 0, kc == 1, r=["OUTN", "Wukk"], w=[PB(C, pb)])
                cp(kb, "act", KT[0:64, hh, blk], C.ps[pb][0:64, :], r=[PB(C, pb)], w=[("KT", b)])
            for q in range(4):
                t = b * 4 + q
                pb = q % 2
                for kc in range(2):
                    mm(kb, C.ps[pb][:, 0:256], CKVN[:, kc, q * 128:(q + 1) * 128], Wukv[:, kc, g * 4:(g + 1) * 4, :], kc == 0, kc == 1, r=["OUTN", "Wukv"], w=[PB(C, pb)])
                cp(kb, "act", V[:, t, :, 0:64], C.ps[pb][:, 0:256].rearrange("p (a b) -> p a b", b=64), r=[PB(C, pb)], w=[("V", b)])
            for hh in range(4):
                h = g * 4 + hh
                for kc in range(3):
                    mm(kb, C.ps[0][0:96, :], Wuq[:, kc, h, :], CQN[:, kc, :], kc == 0, kc == 2, r=["OUTN", "Wuq"], w=[PB(C, 0)])
                for kc in range(3):
                    mm(kb, C.ps[1][0:96, :], Wuqr[:, kc, h, :], CQN[:, kc, :], kc == 0, kc == 2, r=["OUTN", "Wuqr"], w=[PB(C, 1)])
                tt(kb, "dve", TA, C.ps[0][0:96, :], COS, ALU.mult, r=[PB(C, 0), "ROPE"], w=["TA"])
                tt(kb, "dve", TBf, C.ps[1][0:96, :], SIN, ALU.mult, r=[PB(C, 1), "ROPE"], w=["TBf"])
                tt(kb, "pool", QT[:, hh, :], TA, TBf, ALU.add, r=["TA", "TBf"], w=["QT"])
            attn_block(C, b, 4, KT, QT, V, 96, scale, None, "chunk", OTs)
            outproj_block(C, b, 4, OTs, Wo, g == 0)
    kb.barrier()


def dsa_phase(C, j, seq, first):
    kb = C.kb
    W = C.W
    S, NT, NB = C.S, C.NT, C.NB
    kb.barrier()
    kb.sb_off = C.base
    alloc_attn_common(C)
    win = W["ev_w_in"][j]
    Wqb = kb.sb("dWqb", [128, 8, 2, 512], BF16)
    Wkb = kb.sb("dWkb", [128, 8, 2, 64], BF16)
    Wvb = kb.sb("dWvb", [128, 8, 64], BF16)
    Wqi = kb.sb("dWqi", [128, 8, 2, 256], BF16)
    Wki = kb.sb("dWki", [128, 8, 2, 64], BF16)
    Wwi = kb.sb("dWwi", [128, 8, 4], BF16)
    Wo = kb.sb("dWo", [64, 8, 1024], BF16)
    KTb = kb.sb("dKTb", [64, S], BF16)
    Vb = kb.sb("dVb", [128, NT, 65], BF16)
    KTi = kb.sb("dKTi", [64, S], BF16)
    QTb = kb.sb("dQTb", [64, 8, 512], BF16)
    QTi = kb.sb("dQTi", [64, 4, 512], BF16)
    WI = kb.sb("dWI", [128, 4, 4], F32)
    OTs = kb.sb("dOTs", [64, 8, 512], BF16)
    MT = kb.sb("dMT", [128, NT, 128], BF16)
    TAU = kb.sb("dTAU", [128, 8], F32)
    STEPS = kb.sb("dSTEPS", [128, 32], F32)
    offA = kb.sb_off
    COS = kb.sb("dCOS", [64, 512], F32)
    SIN = kb.sb("dSIN", [64, 512], F32)
    TAx = kb.sb("dTA", [128, 512], F32)
    TBx = kb.sb("dTB", [128, 512], F32)
    alloc_rope(C, TAx, TBx)
    TA = TAx[0:64]
    TBf = TBx[0:64]
    endA = kb.sb_off
    kb.sb_off = offA
    SC = kb.sb("dSC", [128, S], F32)
    RL = kb.sb("dRL", [128, 512], F32)
    MASK = kb.sb("dMASK", [128, S], BF16)
    kb.sb_off = max(endA, kb.sb_off)
    psb7 = C.ps[7].bitcast(BF16)

    def cols(c0, c1):
        return win[:, c0:c1].rearrange("(kc p) f -> p kc f", p=128)

    kb.op("pool", lambda e: e.memset(Vb, 1.0), w=[("V", b) for b in range(NB)])
    for h in range(8):
        load_rot(kb, Wqb[:, :, 0, h * 64:(h + 1) * 64], Wqb[:, :, 1, h * 64:(h + 1) * 64], lambda a, b_, h=h: cols(672 + h * 64 + a, 672 + h * 64 + b_), 64, "Wqb")
    load_rot(kb, Wkb[:, :, 0, :], Wkb[:, :, 1, :], lambda a, b_: cols(1184 + a, 1184 + b_), 64, "Wkb")
    wload(kb, Wvb, cols(1248, 1312), "Wvb")
    for h in range(4):
        load_rot(kb, Wqi[:, :, 0, h * 64:(h + 1) * 64], Wqi[:, :, 1, h * 64:(h + 1) * 64], lambda a, b_, h=h: cols(1312 + h * 64 + a, 1312 + h * 64 + b_), 64, "Wqi")
    load_rot(kb, Wki[:, :, 0, :], Wki[:, :, 1, :], lambda a, b_: cols(1568 + a, 1568 + b_), 64, "Wki")
    wload(kb, Wwi, cols(1632, 1636), "Wwi")
    wload(kb, Wo, W["ev_w_o"][j][512:1024, :].rearrange("(h d) n -> d h n", d=64), "Wo")

    def roped(Wt, c0, dst, key_w, wkeys):
        for kc in range(8):
            mm(kb, C.ps[0][0:64, :], Wt[:, kc, 0, c0:c0 + 64], C.XT[:, kc, roped.blk], kc == 0, kc == 7, r=roped.xr + [key_w], w=[PB(C, 0)])
        for kc in range(8):
            mm(kb, C.ps[1][0:64, :], Wt[:, kc, 1, c0:c0 + 64], C.XT[:, kc, roped.blk], kc == 0, kc == 7, r=roped.xr + [key_w], w=[PB(C, 1)])
        tt(kb, "dve", TA, C.ps[0][0:64, :], COS, ALU.mult, r=[PB(C, 0), "ROPE"], w=["TA"])
        tt(kb, "dve", TBf, C.ps[1][0:64, :], SIN, ALU.mult, r=[PB(C, 1), "ROPE"], w=["TBf"])
        tt(kb, "pool", dst, TA, TBf, ALU.add, r=["TA", "TBf"], w=wkeys)

    for b in range(NB):
        blk = slice(b * 512, (b + 1) * 512)
        roped.blk = blk
        roped.xr = [("XT", b * 4 + q) for q in range(4)]
        kb.barrier()
        rope_tables(C, seq, b, [(1, 64, COS, SIN)])
        for h in range(8):
            roped(Wqb, h * 64, QTb[:, h, :], "Wqb", ["QTb"])
        roped(Wkb, 0, KTb[:, blk], "Wkb", [("KTb", b)])
        for h in range(4):
            roped(Wqi, h * 64, QTi[:, h, :], "Wqi", ["QTi"])
        roped(Wki, 0, KTi[:, blk], "Wki", [("KTi", b)])
        for q in range(4):
            t = b * 4 + q
            pb = q % 2
            for kc in range(8):
                mm(kb, C.ps[pb][:, 0:64], C.XT[:, kc, t * 128:(t + 1) * 128], Wvb[:, kc, :], kc == 0, kc == 7, r=[("XT", t), "Wvb"], w=[PB(C, pb)])
            cp(kb, "act", Vb[:, t, 0:64], C.ps[pb][:, 0:64], r=[PB(C, pb)], w=[("V", b)])
            for kc in range(8):
                mm(kb, C.ps[2 + pb][:, 0:4], C.XT[:, kc, t * 128:(t + 1) * 128], Wwi[:, kc, :], kc == 0, kc == 7, r=[("XT", t), "Wwi"], w=[PB(C, 2 + pb)])
            cp(kb, "dve", WI[:, q, :], C.ps[2 + pb][:, 0:4], r=[PB(C, 2 + pb)], w=["WI"])
        kb.barrier()
        for r_ in range(4):
            i = 4 * b + r_
            nk = i + 1
            N2 = nk * 128
            qs = slice(r_ * 128, (r_ + 1) * 128)
            if i >= 2:
                for h in range(4):
                    for c in range((N2 + 511) // 512):
                        n0 = c * 512
                        n1 = min(N2, n0 + 512)
                        bank = C.ps[c % 2]
                        mm(kb, bank[:, 0:n1 - n0], QTi[:, h, qs], KTi[:, n0:n1], True, True, r=["QTi"] + [("KTi", bb) for bb in range(b + 1)], w=[PB(C, c % 2)])
                        act(kb, RL[:, 0:n1 - n0], bank[:, 0:n1 - n0], AF.Relu, r=[PB(C, c % 2)], w=["RL"])
                        stt(kb, SC[:, n0:n1], RL[:, 0:n1 - n0], WI[:, r_, h:h + 1], (C.TB if h == 0 else SC)[:, n0:n1], ALU.mult, ALU.add,
                            r=["RL", "WI", "TB", ("SC", c)], w=[("SC", c)])
                sck = [("SC", c) for c in range((N2 + 511) // 512)]
                tt(kb, "pool", SC[:, i * 128:(i + 1) * 128], SC[:, i * 128:(i + 1) * 128], C.dmask, ALU.add, r=sck + ["dmask"], w=sck)
                red(kb, TAU[:, 0:1], SC[:, 0:N2], ALU.max, r=sck, w=["hi"])
                red(kb, TAU[:, 1:2], SC[:, 0:N2 - 128], ALU.min, r=sck, w=["tau"])
                tt(kb, "dve", TAU[:, 2:3], TAU[:, 0:1], TAU[:, 1:2], ALU.subtract, r=["hi", "tau"], w=["rng"])
                ts(kb, "dve", STEPS[:, 0:NIT + 2], C.pow2[:, 0:NIT + 2], TAU[:, 2:3], None, ALU.mult, None, r=["rng", "pow2"], w=["STEPS"])
                tt(kb, "dve", TAU[:, 3:4], TAU[:, 1:2], STEPS[:, 0:1], ALU.add, r=["tau", "STEPS"], w=["cand"])
                for it in range(NIT):
                    ts(kb, "dve", MASK[:, 0:N2], SC[:, 0:N2], TAU[:, 3:4], None, ALU.is_ge, ALU.add, r=sck + ["cand"], w=["MASK", "cnt"], accum=TAU[:, 4:5])
                    stt(kb, TAU[:, 5:6], TAU[:, 4:5], 255.5, STEPS[:, it:it + 1], ALU.is_ge, ALU.mult, r=["cnt", "STEPS"], w=["inc"])
                    stt(kb, TAU[:, 3:4], TAU[:, 5:6], STEPS[:, it + 1:it + 2], TAU[:, 3:4], ALU.subtract, ALU.add, r=["inc", "STEPS", "cand"], w=["cand"])
                tt(kb, "dve", TAU[:, 1:2], TAU[:, 3:4], STEPS[:, NIT:NIT + 1], ALU.subtract, r=["cand", "STEPS"], w=["tau"])
                ts(kb, "dve", MASK[:, 0:N2], SC[:, 0:N2], TAU[:, 1:2], None, ALU.is_ge, None, r=sck + ["tau"], w=["MASK"])
                for k0 in range(0, nk, 8):
                    k1 = min(nk, k0 + 8)
                    for kt in range(k0, k1):
                        kb.op("pe", lambda e, kt=kt, k0=k0: e.transpose(psb7[:, (kt - k0) * 128:(kt - k0 + 1) * 128], MASK[:, kt * 128:(kt + 1) * 128], C.identb),
                              r=["MASK", "identb"], w=[PB(C, 7)])
                    cp(kb, "act", MT[:, k0:k1, :], psb7[:, 0:(k1 - k0) * 128].rearrange("p (a b) -> p a b", b=128), r=[PB(C, 7)], w=["MT"])
            steps = []
            for hg in range(2):
                otb = 4 + hg
                OTb = C.ps[otb]
                for kt in range(nk):
                    sb_ = C.cnt % 4
                    pi = C.cnt % 4
                    C.cnt += 1

                    def st(hg=hg, kt=kt, sb_=sb_):
                        mm(kb, C.ps[sb_], KTb[:, kt * 128:(kt + 1) * 128], QTb[:, hg * 4:(hg + 1) * 4, qs], True, True, r=[("KTb", kt // 4), "QTb"], w=[PB(C, sb_)])

                    def ex(kt=kt, sb_=sb_, pi=pi):
                        PT3 = C.PT[pi].rearrange("p (a b) -> p a b", b=128)
                        act(kb, C.PT[pi], C.ps[sb_], AF.Exp, r=[PB(C, sb_)], w=[("PT", pi)], scale=0.125)
                        if i >= 2:
                            tt(kb, "dve", PT3, PT3, MT[:, kt:kt + 1, :].broadcast_to([128, 4, 128]), ALU.mult, r=[("PT", pi), "MT"], w=[("PT", pi)])
                        elif kt == i:
                            kb.op("pool", lambda e, PT3=PT3: e.memset(PT3[64:128, :, 0:64], 0.0), r=[("PT", pi)], w=[("PT", pi)])

                    def pv(kt=kt, pi=pi, OTb=OTb, otb=otb):
                        mm(kb, OTb[0:65, :], Vb[:, kt, :], C.PT[pi], kt == 0, kt == nk - 1, r=[("V", kt // 4), ("PT", pi)], w=[PB(C, otb)])

                    sp = dict(st=st, ex=ex, pv=pv)
                    if kt == nk - 1:
                        sp["na"] = lambda OTb=OTb, otb=otb: attn_norm_a(C, OTb, PB(C, otb))
                        sp["nb"] = lambda OTb=OTb, otb=otb, hg=hg: attn_norm_b(C, OTb, PB(C, otb), OTs[:, hg * 4:(hg + 1) * 4, qs], shape3=128)
                    steps.append(sp)
            run_pipelined(steps)
        outproj_block(C, b, 8, OTs, Wo, first)
    kb.barrier()


_CFG = dict(NSEQ=4, S=2048, layers=[0, 1, 2, 3], mla=True, dsa=True)


def kernel(**inputs):
    n = 8
    kb = build(_CFG)
    consts = make_consts()
    x = np.ascontiguousarray(inputs["x"], dtype=np.float32)
    pos = np.ascontiguousarray(inputs["positions"], dtype=np.int32)
    shared = {k: np.ascontiguousarray(v) for k, v in inputs.items() if k not in ("x", "positions")}
    in_maps = []
    for c in range(n):
        m = dict(shared)
        m["x"] = np.ascontiguousarray(x[c * 4:(c + 1) * 4])
        m["positions"] = np.ascontiguousarray(pos[c * 4:(c + 1) * 4])
        m["consts"] = consts
        in_maps.append(m)
    res = run_bass_kernel_spmd(kb.nc, in_maps, core_ids=list(range(n)))
    return np.concatenate([r["out"] for r in res.results], axis=0).astype(np.float32)
```
